# Optimizing a Trainium2 kernel written in Bass

```python
import jax, jax.numpy as jnp
from jax import lax
import numpy as np

D_MODEL = 1024
BATCH = 8
SEQ = 2048
DEPTH = 1

FOX_HEADS = 8
FOX_HEAD_DIM = 64
FOX_WIDTH = FOX_HEADS * FOX_HEAD_DIM
Q_BLOCK = 128
HGRN_HEADS = 8
HGRN_KEY_DIM = 64
HGRN_VAL_DIM = 64
HGRN_WIDTH = HGRN_HEADS * HGRN_KEY_DIM
CHUNK = 64
N_GROUPS = 8
EXPERTS_PER_GROUP = 8
N_EXPERTS = N_GROUPS * EXPERTS_PER_GROUP
TOP_K_IN_GROUP = 2
EXPERT_FF = 256
ALPHA = (2 * DEPTH) ** 0.25
BETA = (8 * DEPTH) ** -0.25
LN_EPS = 1e-5
RMS_EPS = 1e-6
IN_WIDTHS = (FOX_WIDTH, FOX_WIDTH, FOX_WIDTH, FOX_HEADS,
             HGRN_WIDTH, HGRN_WIDTH, HGRN_HEADS * HGRN_VAL_DIM, HGRN_HEADS * HGRN_VAL_DIM,
             D_MODEL, D_MODEL)
IN_DIM = sum(IN_WIDTHS)

kernel_name = "fox_hgrn2_gated_hier_moe_deepnorm_adaln"


def layer_norm(x, g, b):
    xf = x.astype(jnp.float32)
    mu = jnp.mean(xf, axis=-1, keepdims=True)
    var = jnp.mean(jnp.square(xf - mu), axis=-1, keepdims=True)
    return ((xf - mu) * lax.rsqrt(var + LN_EPS) * g + b).astype(x.dtype)


def fox_attention(q, k, v, log_f):
    B, H, S, Dh = q.shape
    cum = jnp.cumsum(log_f, axis=-1)
    scale = Dh ** -0.5
    key_pos = jnp.arange(S)

    def block(i):
        start = i * Q_BLOCK
        qb = lax.dynamic_slice_in_dim(q, start, Q_BLOCK, axis=2)
        cb = lax.dynamic_slice_in_dim(cum, start, Q_BLOCK, axis=2)
        logits = (jnp.einsum('bhqd,bhkd->bhqk', qb, k).astype(jnp.float32) * scale
                  + cb[..., :, None] - cum[..., None, :])
        q_pos = start + jnp.arange(Q_BLOCK)
        logits = jnp.where(q_pos[:, None] >= key_pos[None, :], logits, -jnp.inf)
        p = jax.nn.softmax(logits, axis=-1).astype(v.dtype)
        return jnp.einsum('bhqk,bhkd->bhqd', p, v)

    out = lax.map(block, jnp.arange(S // Q_BLOCK))
    return jnp.moveaxis(out, 0, 2).reshape(B, H, S, Dh)


def hgrn2_chunked(q, k, v, log_f):
    B, H, S, dk = q.shape
    dv = v.shape[-1]
    nc = S // CHUNK

    def to_chunks(a):
        return jnp.moveaxis(a.astype(jnp.float32).reshape(B, H, nc, CHUNK, a.shape[-1]), 2, 0)

    causal = jnp.tril(jnp.ones((CHUNK, CHUNK), dtype=bool))

    def step(state, inp):
        qc, kc, vc, lfc = inp
        b = jnp.cumsum(lfc, axis=2)
        diff = b[:, :, :, None, :] - b[:, :, None, :, :]
        decay = jnp.exp(jnp.where(causal[:, :, None], diff, -jnp.inf))
        scores = jnp.einsum('bhtk,bhsk,bhtsk->bhts', qc, kc, decay)
        intra = jnp.einsum('bhts,bhsv->bhtv', scores, vc)
        inter = jnp.einsum('bhtk,bhkv->bhtv', qc * jnp.exp(b), state)
        b_last = b[:, :, -1, :]
        new_state = (jnp.exp(b_last)[..., None] * state
                     + jnp.einsum('bhsk,bhsv->bhkv', kc * jnp.exp(b_last[:, :, None, :] - b), vc))
        return new_state, intra + inter

    state0 = jnp.zeros((B, H, dk, dv), jnp.float32)
    _, out = lax.scan(step, state0, (to_chunks(q), to_chunks(k), to_chunks(v), to_chunks(log_f)))
    return jnp.moveaxis(out, 0, 2).reshape(B, H, S, dv)


def token_mixer(h, w_in, b_fox_forget, lower_bound, hgrn_norm_w, w_up_fox, w_up_hgrn, w_out):
    B, S, _ = h.shape
    proj = h @ w_in
    offsets = np.cumsum(IN_WIDTHS)[:-1].tolist()
    fq, fk, fv, ff, hq, hf, hi, hg, gate_fox, gate_hgrn = jnp.split(proj, offsets, axis=-1)

    def heads(a, n):
        return a.reshape(B, S, n, -1).transpose(0, 2, 1, 3)

    log_f_fox = jax.nn.log_sigmoid(ff.astype(jnp.float32) + b_fox_forget).transpose(0, 2, 1)
    y_fox = fox_attention(heads(fq, FOX_HEADS), heads(fk, FOX_HEADS), heads(fv, FOX_HEADS), log_f_fox)
    y_fox = y_fox.transpose(0, 2, 1, 3).reshape(B, S, FOX_WIDTH)

    f_h = lower_bound + (1.0 - lower_bound) * jax.nn.sigmoid(hf.astype(jnp.float32))
    q_h = jax.nn.silu(hq)
    o = hgrn2_chunked(heads(q_h, HGRN_HEADS), heads(1.0 - f_h, HGRN_HEADS),
                      heads(hi, HGRN_HEADS), heads(jnp.log(f_h), HGRN_HEADS))
    o = o.transpose(0, 2, 1, 3)
    o = o * lax.rsqrt(jnp.mean(jnp.square(o), axis=-1, keepdims=True) + RMS_EPS)
    o = (o.reshape(B, S, HGRN_HEADS * HGRN_VAL_DIM) * hgrn_norm_w).astype(h.dtype) * jax.nn.silu(hg)

    merged = jax.nn.sigmoid(gate_fox) * (y_fox @ w_up_fox) + jax.nn.sigmoid(gate_hgrn) * (o @ w_up_hgrn)
    return merged @ w_out


def hier_moe(h, w_rg, b_rg, w_re, b_re, w_g, w_u, w_d):
    B, S, D = h.shape
    t = h.reshape(-1, D)
    g_prob = jax.nn.softmax((t @ w_rg).astype(jnp.float32) + b_rg, axis=-1)
    g_w, g_idx = lax.top_k(g_prob, 1)
    e_logits = ((t @ w_re).astype(jnp.float32) + b_re).reshape(-1, N_GROUPS, EXPERTS_PER_GROUP)
    e_in_group = jnp.take_along_axis(e_logits, g_idx[:, :, None], axis=1)[:, 0]
    e_prob = jax.nn.softmax(e_in_group, axis=-1)
    e_w, e_idx = lax.top_k(e_prob, TOP_K_IN_GROUP)
    e_w = e_w / jnp.sum(e_w, axis=-1, keepdims=True) * g_w
    expert_id = g_idx * EXPERTS_PER_GROUP + e_idx
    combine = jnp.sum(jax.nn.one_hot(expert_id, N_EXPERTS, dtype=jnp.float32) * e_w[..., None], axis=1)

    out = jnp.zeros((t.shape[0], D), jnp.float32)
    for grp in range(N_GROUPS):
        sl = slice(grp * EXPERTS_PER_GROUP, (grp + 1) * EXPERTS_PER_GROUP)
        hid = (jax.nn.silu(jnp.einsum('td,edf->tef', t, w_g[sl]))
               * jnp.einsum('td,edf->tef', t, w_u[sl]))
        hid = hid * combine[:, sl, None].astype(hid.dtype)
        out = out + jnp.einsum('tef,efd->td', hid, w_d[sl]).astype(jnp.float32)
    return out.reshape(B, S, D).astype(h.dtype)


def setup_inputs(seed: int = 0) -> dict:
    key = jax.random.key(seed)
    ks = jax.random.split(key, 24)
    D, L = D_MODEL, DEPTH
    nrm = jax.random.normal
    col_scale = jnp.concatenate([
        jnp.full((FOX_WIDTH * 2,), 1.0), jnp.full((FOX_WIDTH,), BETA), jnp.full((FOX_HEADS,), 1.0),
        jnp.full((HGRN_WIDTH * 2,), 1.0), jnp.full((HGRN_HEADS * HGRN_VAL_DIM,), BETA),
        jnp.full((HGRN_HEADS * HGRN_VAL_DIM + 2 * D,), 1.0)])
    return {
        "x": nrm(ks[0], (BATCH, SEQ, D), jnp.float32),
        "c": nrm(ks[1], (BATCH, D), jnp.float32),
        "w_ada": nrm(ks[2], (L, D, 6 * D), jnp.float32) * (0.5 * D ** -0.5),
        "b_ada": nrm(ks[3], (L, 6 * D), jnp.float32) * 0.02,
        "w_in": nrm(ks[4], (L, D, IN_DIM), jnp.float32) * (D ** -0.5) * col_scale,
        "b_fox_forget": 2.0 + 0.5 * nrm(ks[5], (L, FOX_HEADS), jnp.float32),
        "hgrn_lb_logits": 0.5 * nrm(ks[6], (L + 1, HGRN_WIDTH), jnp.float32),
        "hgrn_norm_w": 1.0 + 0.05 * nrm(ks[7], (L, HGRN_HEADS * HGRN_VAL_DIM), jnp.float32),
        "w_up_fox": nrm(ks[8], (L, FOX_WIDTH, D), jnp.float32) * FOX_WIDTH ** -0.5,
        "w_up_hgrn": nrm(ks[9], (L, HGRN_HEADS * HGRN_VAL_DIM, D), jnp.float32) * (HGRN_HEADS * HGRN_VAL_DIM) ** -0.5,
        "w_out": nrm(ks[10], (L, D, D), jnp.float32) * (D ** -0.5) * BETA,
        "ln1_g": 1.0 + 0.05 * nrm(ks[11], (L, D), jnp.float32),
        "ln1_b": 0.02 * nrm(ks[12], (L, D), jnp.float32),
        "w_router_group": nrm(ks[13], (L, D, N_GROUPS), jnp.float32) * D ** -0.5,
        "b_router_group": 0.01 * nrm(ks[14], (L, N_GROUPS), jnp.float32),
        "w_router_expert": nrm(ks[15], (L, D, N_EXPERTS), jnp.float32) * D ** -0.5,
        "b_router_expert": 0.01 * nrm(ks[16], (L, N_EXPERTS), jnp.float32),
        "w_expert_gate": nrm(ks[17], (L, N_EXPERTS, D, EXPERT_FF), jnp.float32) * D ** -0.5,
        "w_expert_up": nrm(ks[18], (L, N_EXPERTS, D, EXPERT_FF), jnp.float32) * D ** -0.5,
        "w_expert_down": nrm(ks[19], (L, N_EXPERTS, EXPERT_FF, D), jnp.float32) * (EXPERT_FF ** -0.5) * BETA,
        "ln2_g": 1.0 + 0.05 * nrm(ks[20], (L, D), jnp.float32),
        "ln2_b": 0.02 * nrm(ks[21], (L, D), jnp.float32),
    }


def reference(x, c, w_ada, b_ada, w_in, b_fox_forget, hgrn_lb_logits, hgrn_norm_w, w_up_fox, w_up_hgrn,
              w_out, ln1_g, ln1_b, w_router_group, b_router_group, w_router_expert, b_router_expert,
              w_expert_gate, w_expert_up, w_expert_down, ln2_g, ln2_b):
    lower_bounds = jnp.cumsum(jax.nn.softmax(hgrn_lb_logits.astype(jnp.float32), axis=0), axis=0)
    c_act = jax.nn.silu(c)
    for l in range(DEPTH):
        ada = c_act @ w_ada[l] + b_ada[l]
        sh1, sc1, g1, sh2, sc2, g2 = jnp.split(ada, 6, axis=-1)
        h = x * (1.0 + sc1[:, None, :]) + sh1[:, None, :]
        y = token_mixer(h, w_in[l], b_fox_forget[l], lower_bounds[l], hgrn_norm_w[l],
                        w_up_fox[l], w_up_hgrn[l], w_out[l])
        x = layer_norm(ALPHA * x + g1[:, None, :] * y, ln1_g[l], ln1_b[l])
        h = x * (1.0 + sc2[:, None, :]) + sh2[:, None, :]
        y = hier_moe(h, w_router_group[l], b_router_group[l], w_router_expert[l], b_router_expert[l],
                     w_expert_gate[l], w_expert_up[l], w_expert_down[l])
        x = layer_norm(ALPHA * x + g2[:, None, :] * y, ln2_g[l], ln2_b[l])
    return x
```

```python
import bisect
from contextlib import ExitStack, suppress
import numpy as np
import concourse.bass as bass
import concourse.mybir as mybir
from concourse.bass_utils import run_bass_kernel_spmd

F32 = mybir.dt.float32
BF16 = mybir.dt.bfloat16
AF = mybir.ActivationFunctionType
ALU = mybir.AluOpType
AX = mybir.AxisListType

S = 2048
D = 1024
NT = 16
IN_DIM = 5640
O_FQ, O_FK, O_FV, O_FF, O_HQ, O_HF, O_HI, O_HG, O_GF, O_GH = 0, 512, 1024, 1536, 1544, 2056, 2568, 3080, 3592, 4616
ALPHA = 2.0 ** 0.25
LN_EPS = 1e-5
RMS_EPS = 1e-6
NEXP = 64
BIG = 1.0e4


class Tok:
    __slots__ = ("eng", "idx")

    def __init__(self, eng, idx):
        self.eng = eng
        self.idx = idx


class DTok:
    __slots__ = ("sem", "val", "key")

    def __init__(self, sem, val, key):
        self.sem = sem
        self.val = val
        self.key = key


class Eng:
    def __init__(self, K, name, e, self_sync=True):
        self.K = K
        self.name = name
        self.e = e
        self.sem = K.root.enter_context(K.nc.semaphore("s_" + name))
        self.n = 0
        self.cnt = 0
        self.last = None
        self.sig_idx = []
        self.sig_val = []
        self.seen = {}
        self.self_sync = self_sync

    def emit(self, ins):
        self.n += 1
        self.last = ins
        return Tok(self, self.n)

    def value_for(self, idx):
        p = bisect.bisect_left(self.sig_idx, idx)
        if p < len(self.sig_idx):
            return self.sig_val[p]
        assert self.last is not None and self.n >= idx
        self.last.then_inc(self.sem, 1)
        self.cnt += 1
        self.sig_idx.append(self.n)
        self.sig_val.append(self.cnt)
        return self.cnt

    def signal_last(self):
        if self.last is not None and (not self.sig_idx or self.sig_idx[-1] != self.n):
            self.last.then_inc(self.sem, 1)
            self.cnt += 1
            self.sig_idx.append(self.n)
            self.sig_val.append(self.cnt)

    def wait(self, tok):
        if tok is None:
            return
        if isinstance(tok, DTok):
            if self.seen.get(tok.key, 0) >= tok.val:
                return
            self.e.wait_ge(tok.sem, tok.val)
            self.seen[tok.key] = tok.val
            return
        if tok.eng is self and not self.self_sync:
            return
        src = tok.eng
        p = bisect.bisect_left(src.sig_idx, tok.idx)
        if p < len(src.sig_idx) and self.seen.get(src.name, 0) >= src.sig_val[p]:
            return
        v = src.value_for(tok.idx)
        if self.seen.get(src.name, 0) >= v:
            return
        self.e.wait_ge(src.sem, v)
        self.seen[src.name] = v


class Buf:
    def __init__(self, name=""):
        self.name = name
        self.w = None
        self.wl = []
        self.r = {}
        self.rd = []


class DmaQ:
    def __init__(self, K, name, issuer, nsem):
        self.K = K
        self.name = name
        self.I = issuer
        self.sems = [K.root.enter_context(K.nc.semaphore(f"d_{name}{i}")) for i in range(nsem)]
        self.vals = [0] * nsem
        self.rr = 0
        self.out = []

    def dma(self, out, in_, outs=(), ins=()):
        I = self.I
        for b in ins:
            I.wait(b.w)
            for t in b.wl:
                I.wait(t)
        for b in outs:
            I.wait(b.w)
            for t in b.wl:
                I.wait(t)
            for t in b.r.values():
                I.wait(t)
            for t in b.rd:
                I.wait(t)
        j = self.rr
        self.rr = (self.rr + 1) % len(self.sems)
        key = f"{self.name}{j}"
        if self.vals[j] > 0:
            I.wait(DTok(self.sems[j], self.vals[j], key))
        ins_ = I.e.dma_start(out=out, in_=in_)
        ins_.then_inc(self.sems[j], 16)
        self.vals[j] += 16
        tok = DTok(self.sems[j], self.vals[j], key)
        for b in ins:
            b.rd.append(tok)
        for b in outs:
            if isinstance(b.w, DTok):
                b.wl.append(b.w)
            b.w = tok
            b.r = {}
            b.rd = []
        return tok

    def all_toks(self):
        return [DTok(self.sems[j], self.vals[j], f"{self.name}{j}") for j in range(len(self.sems)) if self.vals[j] > 0]


class Kern:
    def __init__(self, nc, root):
        self.nc = nc
        self.root = root
        self.es = root
        self.pe = Eng(self, "pe", nc.tensor, self_sync=False)
        self.act = Eng(self, "act", nc.scalar)
        self.dve = Eng(self, "dve", nc.vector)
        self.pool = Eng(self, "pool", nc.gpsimd)
        self.sp = Eng(self, "sp", nc.sync)
        self.engs = [self.pe, self.act, self.dve, self.pool, self.sp]
        self.q_sp = DmaQ(self, "qsp", self.sp, 8)
        self.q_pl = DmaQ(self, "qpl", self.pool, 12)
        self.nbuf = 0
        nbytes = (int(nc.sbuf_bytes_remaining) - 2048) // 64 * 64
        self.arena = root.enter_context(nc.sbuf_tensor("arena", [128, nbytes // 4], F32))
        self.free = [(0, nbytes)]
        self.scopes = [[]]
        self.peak = 0
        self.nbytes = nbytes

    def _alloc(self, n):
        for idx, (o, sz) in enumerate(self.free):
            if sz >= n:
                if sz == n:
                    self.free.pop(idx)
                else:
                    self.free[idx] = (o + n, sz - n)
                used = self.nbytes - sum(z for _, z in self.free)
                self.peak = max(self.peak, used)
                return o
        raise RuntimeError(f"arena OOM need {n} free {self.free}")

    def _release(self, o, n):
        self.free.append((o, n))
        self.free.sort()
        m = []
        for o_, n_ in self.free:
            if m and m[-1][0] + m[-1][1] == o_:
                m[-1] = (m[-1][0], m[-1][1] + n_)
            else:
                m.append((o_, n_))
        self.free = m

    def sb(self, name, shape, dt, persist=False):
        parts = shape[0]
        elems = 1
        for d in shape[1:]:
            elems *= d
        esz = 2 if dt == BF16 else 4
        n = (elems * esz + 63) // 64 * 64
        o = self._alloc(n)
        v = self.arena[0:parts, o // 4:(o + n) // 4]
        if dt != F32:
            v = v.bitcast(dt)
        v = v[:, 0:elems]
        if len(shape) > 2:
            names = "abcdefg"[:len(shape) - 1]
            pat = "p (" + " ".join(names) + ") -> p " + " ".join(names)
            v = v.rearrange(pat, **{names[i]: shape[1 + i] for i in range(len(shape) - 1)})
        if persist:
            self.handles = getattr(self, "handles", {})
            self.handles[name] = (o, n)
        else:
            self.scopes[-1].append((o, n))
        return v

    def release(self, name):
        o, n = self.handles.pop(name)
        self._release(o, n)

    def push(self):
        self.scopes.append([])

    def pop(self):
        for o, n in self.scopes.pop():
            self._release(o, n)

    def op(self, eng, fn, outs=(), ins=()):
        for b in ins:
            eng.wait(b.w)
            for t in b.wl:
                eng.wait(t)
        for b in outs:
            eng.wait(b.w)
            for t in b.wl:
                eng.wait(t)
            for t in b.r.values():
                eng.wait(t)
            for t in b.rd:
                eng.wait(t)
        if eng is self.pe:
            key = tuple(id(b) for b in outs)
            if key != getattr(eng, "prev_outs", None):
                eng.signal_last()
            eng.prev_outs = key
        tok = eng.emit(fn())
        if eng is not self.pe and eng is not self.sp:
            eng.signal_last()
        for b in ins:
            b.r[eng.name] = tok
        for b in outs:
            b.w = tok
            b.wl = []
            b.r = {}
            b.rd = []
        return tok

    def barrier(self):
        toks = [Tok(e, e.n) for e in self.engs if e.n > 0]
        dt = self.q_sp.all_toks() + self.q_pl.all_toks()
        for e in self.engs:
            for t in toks:
                if t.eng is not e:
                    e.wait(t)
            for t in dt:
                e.wait(t)


class _Stop(Exception):
    pass


def build(debug=None, stop=None):
    debug = debug or []
    nc = bass.Bass("TRN2", target_bir_lowering=False)

    def din(name, shape):
        return nc.dram_tensor(name, shape, F32, kind="ExternalInput").ap()

    x_d = din("x", [S, D])
    cT_d = din("cT", [128, 8])
    wada_d = din("w_ada", [D, 6 * D])
    badaT_d = din("b_adaT", [128, 48])
    win_d = din("w_in", [D, IN_DIM])
    bff_d = din("bff", [8])
    lbl_d = din("lbl", [128, 8])
    nw_d = din("nw", [512])
    wupf_d = din("w_up_fox", [512, D])
    wuph_d = din("w_up_hgrn", [512, D])
    wout_d = din("w_out", [D, D])
    ln1g_d = din("ln1_g", [D])
    ln1b_d = din("ln1_b", [D])
    ln2g_d = din("ln2_g", [D])
    ln2b_d = din("ln2_b", [D])
    wr_d = din("w_r", [D, 72])
    br_d = din("b_r", [72])
    nexp_decl = 1 if (stop is not None and stop < 7) else NEXP
    weg_d = din("w_eg", [nexp_decl * 128, 2048])
    weu_d = din("w_eu", [nexp_decl * 128, 2048])
    wed_d = din("w_ed", [nexp_decl * 128, 2048])
    out_d = nc.dram_tensor("out", [S, D], F32, kind="ExternalOutput").ap()
    x1_d = nc.dram_tensor("x1_scratch", [S, D], F32, kind="Internal").ap()

    dbg_out = {}

    with ExitStack() as root, suppress(_Stop):
        K = Kern(nc, root)
        op = K.op
        PEe, ACT, DVE, POOL = K.pe, K.act, K.dve, K.pool
        te, se, ve, ge = nc.tensor, nc.scalar, nc.vector, nc.gpsimd

        def dump(name, ap, shape, bufs, dt=F32):
            if name not in debug:
                return
            d = nc.dram_tensor("dbg_" + name, list(shape), dt, kind="ExternalOutput").ap()
            dbg_out[name] = K.q_sp.dma(d, ap, ins=bufs)
            K.sp.wait(dbg_out[name])

        PB = [root.enter_context(nc.psum_tensor(f"pb{i}", [128, 512], F32)) for i in range(8)]
        BPB = [Buf(f"pb{i}") for i in range(8)]
        bank_rr = [0]

        def nbank(lo=0, hi=8):
            n = hi - lo
            i = lo + bank_rr[0] % n
            bank_rr[0] += 1
            return i

        Bc = Buf("const")
        ident_f = K.sb("ident_f", [128, 128], F32)
        ident_b = K.sb("ident_b", [128, 128], BF16)
        ones_f = K.sb("ones_f", [128, 128], F32)
        tri_f = K.sb("tri_f", [128, 128], F32)
        negmask_b = K.sb("negmask_b", [128, 128], BF16)
        hmask_f = K.sb("hmask_f", [128, 128], F32)
        rmask = K.sb("rmask", [128, S], F32)
        op(POOL, lambda: ge.memset(ident_f[:], 0.0), outs=[Bc])
        op(POOL, lambda: ge.affine_select(out=ident_f[:], in_=ident_f[:], compare_op=ALU.not_equal, fill=1.0,
                                          base=0, pattern=[[-1, 128]], channel_multiplier=1), outs=[Bc])
        op(POOL, lambda: ge.tensor_copy(out=ident_b[:], in_=ident_f[:]), outs=[Bc])
        op(POOL, lambda: ge.memset(ones_f[:], 1.0), outs=[Bc])
        op(POOL, lambda: ge.memset(tri_f[:], 1.0), outs=[Bc])
        op(POOL, lambda: ge.affine_select(out=tri_f[:], in_=tri_f[:], compare_op=ALU.is_ge, fill=0.0,
                                          base=0, pattern=[[1, 128]], channel_multiplier=-1), outs=[Bc])
        op(POOL, lambda: ge.memset(negmask_b[:], 0.0), outs=[Bc])
        op(POOL, lambda: ge.affine_select(out=negmask_b[:], in_=negmask_b[:], compare_op=ALU.is_ge, fill=-30000.0,
                                          base=0, pattern=[[1, 128]], channel_multiplier=-1), outs=[Bc])
        op(POOL, lambda: ge.tensor_copy(out=hmask_f[:], in_=tri_f[:]), outs=[Bc])
        op(POOL, lambda: ge.memset(hmask_f[0:64, 64:128], 0.0), outs=[Bc])
        op(POOL, lambda: ge.memset(rmask[:], 1.0), outs=[Bc])
        op(POOL, lambda: ge.memset(rmask[:, 0:S:64], 0.0), outs=[Bc])

        win_r = win_d.rearrange("(k p) n -> p k n", p=128)
        wq = K.sb("wq", [128, 8, 8, 65], BF16, persist=True)
        wk = K.sb("wk", [128, 8, 512], BF16, persist=True)
        wv = K.sb("wv", [128, 8, 512], BF16, persist=True)
        wff = K.sb("wff", [128, 8, 8], BF16, persist=True)
        Bw = Buf("wA")
        K.q_pl.dma(wv[:], win_r[:, :, O_FV:O_FV + 512], outs=[Bw])
        K.q_pl.dma(wff[:], win_r[:, :, O_FF:O_FF + 8], outs=[Bw])
        for k in range(8):
            K.q_pl.dma(wq[:, k, :, 0:64], win_r[:, k, O_FQ:O_FQ + 512].rearrange("p (h d) -> p h d", h=8), outs=[Bw])
        op(POOL, lambda: ge.memset(wq[:, :, :, 64:65], 0.0), outs=[Bw])
        K.q_pl.dma(wk[:], win_r[:, :, O_FK:O_FK + 512], outs=[Bw])

        Bada = Buf("ada")
        adaT = K.sb("adaT", [128, 48], F32)
        sc1p = K.sb("sc1p", [128, 8], F32)
        sc2p = K.sb("sc2p", [128, 8], F32)
        lb = K.sb("lb", [128, 4], F32)
        omlb = K.sb("omlb", [128, 4], F32)
        nomlb = K.sb("nomlb", [128, 4], F32)
        bffb = K.sb("bffb", [128, 8], F32)
        if True:
            K.push()
            cT_sb = K.sb("cT_sb", [128, 8], F32)
            c_act = K.sb("c_act", [128, 8], F32)
            badaT = K.sb("badaT", [128, 48], F32)
            lbl = K.sb("lbl_sb", [128, 8], F32)
            wa = [K.sb(f"wa{i}", [128, 8, 512], BF16) for i in range(3)]
            c_actb = K.sb("c_actb", [128, 8], BF16)
            Bwa = [Buf(), Buf(), Buf()]
            Bs = Buf("small")
            K.q_sp.dma(cT_sb[:], cT_d, outs=[Bs])
            K.q_sp.dma(badaT[:], badaT_d, outs=[Bs])
            K.q_sp.dma(lbl[:], lbl_d, outs=[Bs])
            K.q_sp.dma(bffb[:], bff_d.partition_broadcast(128), outs=[Bs])
            op(ACT, lambda: se.activation(out=c_act[:], in_=cT_sb[:], func=AF.Silu), outs=[Bs], ins=[Bs])
            op(ACT, lambda: se.copy(out=c_actb[:], in_=c_act[:]), outs=[Bs], ins=[Bs])
            op(DVE, lambda: ve.tensor_tensor(out=lb[:], in0=lbl[:, 0:4], in1=lbl[:, 4:8], op=ALU.subtract), outs=[Bada], ins=[Bs])
            op(ACT, lambda: se.activation(out=lb[:], in_=lb[:], func=AF.Sigmoid), outs=[Bada], ins=[Bada])
            op(DVE, lambda: ve.tensor_scalar(out=omlb[:], in0=lb[:], scalar1=-1.0, scalar2=1.0, op0=ALU.mult, op1=ALU.add), outs=[Bada], ins=[Bada])
            op(DVE, lambda: ve.tensor_scalar(out=nomlb[:], in0=lb[:], scalar1=1.0, scalar2=None, op0=ALU.subtract), outs=[Bada], ins=[Bada])
            wada_r = wada_d.rearrange("(k p) n -> p k n", p=128)
            pa = 0
            for cb in range(12):
                w_ = wa[cb % 3]
                K.q_pl.dma(w_[:], wada_r[:, :, cb * 512:(cb + 1) * 512], outs=[Bwa[cb % 3]])
                for jj in range(4):
                    j = cb * 4 + jj
                    for k in range(8):
                        op(PEe, lambda: te.matmul(PB[pa][:, j:j + 1], lhsT=w_[:, k, jj * 128:(jj + 1) * 128],
                                                  rhs=c_actb[:, k:k + 1], start=(k == 0), stop=(k == 7)),
                           outs=[BPB[pa]], ins=[Bwa[cb % 3], Bs])
            op(DVE, lambda: ve.tensor_tensor(out=adaT[:], in0=PB[pa][:, 0:48], in1=badaT[:], op=ALU.add), outs=[Bada], ins=[BPB[pa], Bs])
            op(DVE, lambda: ve.tensor_scalar(out=sc1p[:], in0=adaT[:, 8:16], scalar1=1.0, scalar2=None, op0=ALU.add), outs=[Bada], ins=[Bada])
            op(DVE, lambda: ve.tensor_scalar(out=sc2p[:], in0=adaT[:, 32:40], scalar1=1.0, scalar2=None, op0=ALU.add), outs=[Bada], ins=[Bada])
            dump("adaT", adaT[:], [128, 48], [Bada])
            K.barrier()
            K.pop()
            if stop == 0:
                raise _Stop()

        def bcast_rows(dst, Bdst, colsrc):
            for half in range(2):
                dg = K.sb("dg", [128, 512], F32)
                Bdg = Buf()
                for jj in range(4):
                    j = half * 4 + jj
                    op(DVE, lambda: ve.tensor_scalar(out=dg[:, jj * 128:(jj + 1) * 128], in0=ident_f[:], scalar1=colsrc[:, j:j + 1], scalar2=None,
                                                     op0=ALU.mult), outs=[Bdg], ins=[Bc, Bada])
                bk = nbank(0, 8)
                op(PEe, lambda: te.matmul(PB[bk][:, :], lhsT=ones_f[:], rhs=dg[:], start=True, stop=True), outs=[BPB[bk]], ins=[Bdg, Bc])
                op(ACT, lambda: se.copy(out=dst[:, half * 512:(half + 1) * 512], in_=PB[bk][:, :]), outs=[Bdst], ins=[BPB[bk]])

        hT = K.sb("hT", [128, 8, S], BF16, persist=True)
        BhT = [Buf(f"hT{i}") for i in range(NT)]
        if True:
            K.push()
            SC1row = K.sb("SC1row", [128, D], F32)
            SH1row = K.sb("SH1row", [128, D], F32)
            Brow1 = Buf("rows_s1")
            K.push()
            bcast_rows(SC1row, Brow1, sc1p)
            bcast_rows(SH1row, Brow1, adaT[:, 0:8])
            K.barrier()
            K.pop()
            xin = [K.sb(f"xin{i}", [128, D], F32) for i in range(3)]
            Bxin = [Buf(), Buf(), Buf()]
            hm = [K.sb(f"hm{i}", [128, D], F32) for i in range(2)]
            Bhm = [Buf(), Buf()]
            hb = [K.sb(f"hb{i}", [128, D], BF16) for i in range(2)]
            Bhb = [Buf(), Buf()]
            for i in range(2):
                K.q_sp.dma(xin[i][:], x_d[i * 128:(i + 1) * 128, :], outs=[Bxin[i]])
            for i in range(NT):
                p = i % 2
                xb = xin[i % 3]
                if i + 2 < NT:
                    K.q_sp.dma(xin[(i + 2) % 3][:], x_d[(i + 2) * 128:(i + 3) * 128, :], outs=[Bxin[(i + 2) % 3]])
                op(DVE, lambda: ve.tensor_tensor(out=hm[p][:], in0=xb[:], in1=SC1row[:], op=ALU.mult), outs=[Bhm[p]], ins=[Bxin[i % 3], Brow1])
                op(DVE, lambda: ve.tensor_tensor(out=hb[p][:], in0=hm[p][:], in1=SH1row[:], op=ALU.add), outs=[Bhb[p]], ins=[Bhm[p], Brow1])
                pbf = PB[p][:, :].bitcast(BF16)
                for c in range(8):
                    op(PEe, lambda: te.transpose(pbf[:, c * 128:(c + 1) * 128], hb[p][:, c * 128:(c + 1) * 128], ident_b[:]),
                       outs=[BPB[p]], ins=[Bhb[p], Bc])
                op(ACT, lambda: se.copy(out=hT[:, :, i * 128:(i + 1) * 128], in_=pbf[:, :].rearrange("p (c t) -> p c t", c=8)),
                   outs=[BhT[i]], ins=[BPB[p]])
            dump("hT", hT[:], [128, 8, S], BhT, BF16)
            K.barrier()
            K.pop()
            if stop == 1:
                raise _Stop()


        def hT_blk(tb):
            return BhT[4 * tb:4 * tb + 4]

        yfoxT = K.sb("yfoxT", [128, 4, S], BF16, persist=True)
        ByfT = Buf("yfoxT")
        if True:
            K.push()
            v_aug = K.sb("v_aug", [128, NT, 8, 65], BF16)
            Bv = Buf("v_aug")
            op(POOL, lambda: ge.memset(v_aug[:, :, :, 64:65], 1.0), outs=[Bv])
            FFB = 7
            for i in range(NT):
                bk = nbank(0, 4)
                for k in range(8):
                    op(PEe, lambda: te.matmul(PB[bk][:, :], lhsT=hT[:, k, i * 128:(i + 1) * 128], rhs=wv[:, k, :], start=(k == 0), stop=(k == 7)),
                       outs=[BPB[bk]], ins=[BhT[i], Bw])
                src = PB[bk][:, :].rearrange("p (h d) -> p h d", h=8)
                if i % 2 == 0:
                    op(ACT, lambda: se.copy(out=v_aug[:, i, :, 0:64], in_=src), outs=[Bv], ins=[BPB[bk]])
                else:
                    op(DVE, lambda: ve.tensor_copy(out=v_aug[:, i, :, 0:64], in_=src), outs=[Bv], ins=[BPB[bk]])
                for k in range(8):
                    op(PEe, lambda: te.matmul(PB[FFB][:, i * 8:(i + 1) * 8], lhsT=hT[:, k, i * 128:(i + 1) * 128], rhs=wff[:, k, :], start=(k == 0), stop=(k == 7)),
                       outs=[BPB[FFB]], ins=[BhT[i], Bw])
            if stop == 1.1:
                K.barrier()
                raise _Stop()
            lfn = K.sb("lfn", [128, NT, 8], F32)
            Blf = Buf("lf")
            negcum = K.sb("negcum", [128, NT, 8], F32)
            carry = K.sb("carry", [128, NT, 8], F32)
            tot = K.sb("tot", [128, NT, 8], F32)
            Bnc = Buf("negcum")
            op(DVE, lambda: ve.tensor_tensor(out=lfn[:], in0=PB[FFB][:, 0:128].rearrange("p (i h) -> p i h", h=8),
                                             in1=bffb[:].unsqueeze(1).to_broadcast([128, NT, 8]), op=ALU.add), outs=[Blf], ins=[BPB[FFB], Bada])
            op(ACT, lambda: se.activation(out=lfn[:], in_=lfn[:], func=AF.Exp, scale=-1.0), outs=[Blf], ins=[Blf])
            op(ACT, lambda: se.activation(out=lfn[:], in_=lfn[:], func=AF.Ln, bias=1.0, scale=1.0), outs=[Blf], ins=[Blf])
            lfn2 = lfn[:].rearrange("p i h -> p (i h)")
            op(PEe, lambda: te.matmul(PB[4][:, 0:128], lhsT=tri_f[:], rhs=lfn2, start=True, stop=True), outs=[BPB[4]], ins=[Blf, Bc])
            op(PEe, lambda: te.matmul(PB[5][:, 0:128], lhsT=ones_f[:], rhs=lfn2, start=True, stop=True), outs=[BPB[5]], ins=[Blf, Bc])
            Bcar = Buf("carry")
            op(DVE, lambda: ve.tensor_copy(out=tot[:].rearrange("p i h -> p (i h)"), in_=PB[5][:, 0:128]), outs=[Bcar], ins=[BPB[5]])
            op(DVE, lambda: ve.memset(carry[:, 0, :], 0.0), outs=[Bcar], ins=[Bcar])
            for i in range(1, NT):
                op(DVE, lambda: ve.tensor_tensor(out=carry[:, i, :], in0=carry[:, i - 1, :], in1=tot[:, i - 1, :], op=ALU.add), outs=[Bcar], ins=[Bcar])
            op(DVE, lambda: ve.tensor_tensor(out=negcum[:].rearrange("p i h -> p (i h)"), in0=PB[4][:, 0:128],
                                             in1=carry[:].rearrange("p i h -> p (i h)"), op=ALU.add), outs=[Bnc], ins=[BPB[4], Bcar])
            dump("negcum", negcum[:], [128, NT, 8], [Bnc])
            if stop == 1.2:
                K.barrier()
                raise _Stop()
            Zr = K.sb("Zr", [128, NT, 8, 65], BF16)
            BZr = Buf("Zr")
            op(POOL, lambda: ge.memset(Zr[:], 0.0), outs=[BZr])
            op(DVE, lambda: ve.tensor_scalar(out=Zr[:, :, :, 64:65], in0=negcum[:].unsqueeze(3), scalar1=-1.0, scalar2=None, op0=ALU.mult),
               outs=[BZr], ins=[Bnc, BZr])
            qscale = K.sb("qscale", [65, 1], F32)
            Bqs = Buf("qscale")
            op(POOL, lambda: ge.memset(qscale[:], 1.0), outs=[Bqs])
            op(POOL, lambda: ge.memset(qscale[0:64, :], 0.125), outs=[Bqs])
            if stop == 1.3:
                K.barrier()
                raise _Stop()
            qa = [K.sb(f"qa{i}", [65, S], BF16) for i in range(2)]
            ka = [K.sb(f"ka{i}", [65, S], BF16) for i in range(2)]
            Bqa = [Buf(), Buf()]
            Bka = [Buf(), Buf()]
            for i in range(2):
                op(POOL, lambda: ge.memset(ka[i][:], 1.0), outs=[Bka[i]])
            yfox = K.sb("yfox", [128, NT, 512], BF16)
            Byf = [Buf(f"yf{i}") for i in range(NT)]
            pts = [K.sb(f"pt{i}", [128, 512], BF16) for i in range(3)]
            Bpt = [Buf() for _ in range(3)]
            rec = K.sb("rec", [128, 8], F32)
            Brec = Buf("rec")
            ptn = [0]
            SB_ = [0, 1, 2, 3]
            ACCB = [4, 5]
            Bacc = [[Buf() for _ in range(4)] for _ in range(2)]
            def proj_groups(h):
                q_, k_ = qa[h % 2], ka[h % 2]
                Bq, Bk = Bqa[h % 2], Bka[h % 2]
                gs = []
                for tb in range(4):
                    def gq(tb=tb):
                        bk = 6 + (tb % 2)
                        for k in range(8):
                            op(PEe, lambda: te.matmul(PB[bk][0:65, :], lhsT=wq[:, k, h, :], rhs=hT[:, k, tb * 512:(tb + 1) * 512],
                                                      start=(k == 0), stop=False), outs=[BPB[bk]], ins=hT_blk(tb) + [Bw])
                        for ii in range(4):
                            op(PEe, lambda: te.matmul(PB[bk][0:65, ii * 128:(ii + 1) * 128], lhsT=Zr[:, tb * 4 + ii, h, :], rhs=ident_b[:],
                                                      start=False, stop=(ii == 3)), outs=[BPB[bk]], ins=[BZr, Bc])
                        op(DVE, lambda: ve.tensor_scalar(out=q_[0:65, tb * 512:(tb + 1) * 512], in0=PB[bk][0:65, :], scalar1=qscale[:, 0:1],
                                                         scalar2=None, op0=ALU.mult), outs=[Bq], ins=[BPB[bk], Bqs])

                    def gk(tb=tb):
                        bk2 = 6 + ((tb + 1) % 2)
                        for k in range(8):
                            op(PEe, lambda: te.matmul(PB[bk2][0:64, :], lhsT=wk[:, k, h * 64:(h + 1) * 64], rhs=hT[:, k, tb * 512:(tb + 1) * 512],
                                                      start=(k == 0), stop=(k == 7)), outs=[BPB[bk2]], ins=hT_blk(tb) + [Bw])
                        op(DVE, lambda: ve.tensor_copy(out=k_[0:64, tb * 512:(tb + 1) * 512], in_=PB[bk2][0:64, :]), outs=[Bk], ins=[BPB[bk2]])
                    gs += [gq, gk]
                return gs

            for g_ in proj_groups(0):
                g_()
            for h in range(8):
                q_, k_ = qa[h % 2], ka[h % 2]
                Bq, Bk = Bqa[h % 2], Bka[h % 2]
                pending = proj_groups(h + 1) if h + 1 < 8 else []
                itc = [0]

                if stop == 1.4:
                    K.barrier()
                    raise _Stop()

                def qk(I, j):
                    jj = j - 4 * I
                    c0 = max(jj, 0) * 128
                    N = 512 - c0
                    bk = SB_[nbank(0, 4)]
                    ksl = k_[0:65, j * 128:(j + 1) * 128]
                    if jj < 0:
                        op(PEe, lambda: te.matmul(PB[bk][:, 0:N], lhsT=ksl, rhs=q_[0:65, I * 512 + c0:(I + 1) * 512], start=True, stop=True),
                           outs=[BPB[bk]], ins=[Bq, Bk])
                    else:
                        op(PEe, lambda: te.matmul(PB[bk][:, 0:128], lhsT=ksl, rhs=q_[0:65, I * 512 + c0:I * 512 + c0 + 128], start=True, stop=False),
                           outs=[BPB[bk]], ins=[Bq, Bk])
                        op(PEe, lambda: te.matmul(PB[bk][:, 0:128], lhsT=ident_b[:], rhs=negmask_b[:], start=False, stop=True),
                           outs=[BPB[bk]], ins=[Bc])
                        if N > 128:
                            op(PEe, lambda: te.matmul(PB[bk][:, 128:N], lhsT=ksl, rhs=q_[0:65, I * 512 + c0 + 128:(I + 1) * 512], start=True, stop=True),
                               outs=[BPB[bk]], ins=[Bq, Bk])
                    return bk, c0, N, jj

                for I in range(4):
                    ab = ACCB[I % 2]
                    Ba = Bacc[I % 2]
                    nj = 4 * I + 4
                    pend = qk(I, 0)
                    for j in range(nj):
                        bk, c0, N, jj = pend
                        if j + 1 < nj:
                            pend = qk(I, j + 1)
                        pi = ptn[0] % 3
                        ptn[0] += 1
                        pt = pts[pi]
                        op(ACT, lambda: se.activation(out=pt[:, 0:N], in_=PB[bk][:, 0:N], func=AF.Exp, bias=negcum[:, j, h:h + 1], scale=1.0),
                           outs=[Bpt[pi]], ins=[BPB[bk], Bnc])
                        for ii in range(max(jj, 0), 4):
                            i = 4 * I + ii
                            op(PEe, lambda: te.matmul(PB[ab][:, ii * 65:(ii + 1) * 65], lhsT=pt[:, ii * 128 - c0:ii * 128 - c0 + 128],
                                                      rhs=v_aug[:, j, h, :], start=(j == 0 and ii == 0), stop=(j == i), skip_group_check=True),
                               outs=[BPB[ab]], ins=[Bpt[pi], Bv])
                        itc[0] += 1
                        if itc[0] % 5 == 0 and pending:
                            pending.pop(0)()
                    for ii in range(4):
                        i = 4 * I + ii
                        op(DVE, lambda: ve.reciprocal(out=rec[:, ii:ii + 1], in_=PB[ab][:, ii * 65 + 64:ii * 65 + 65]), outs=[Brec], ins=[BPB[ab]])
                        op(DVE, lambda: ve.tensor_scalar(out=yfox[:, i, h * 64:(h + 1) * 64], in0=PB[ab][:, ii * 65:ii * 65 + 64],
                                                         scalar1=rec[:, ii:ii + 1], scalar2=None, op0=ALU.mult), outs=[Byf[i]], ins=[BPB[ab], Brec])
                    if I == 3:
                        while pending:
                            pending.pop(0)()
            if stop == 1.5:
                K.barrier()
                raise _Stop()
            dump("yfox", yfox[:], [128, NT, 512], Byf, BF16)
            for i in range(NT):
                bk = nbank(0, 4)
                pbf = PB[bk][:, :].bitcast(BF16)
                for c4 in range(4):
                    op(PEe, lambda: te.transpose(pbf[:, c4 * 128:(c4 + 1) * 128], yfox[:, i, c4 * 128:(c4 + 1) * 128], ident_b[:]),
                       outs=[BPB[bk]], ins=[Byf[i], Bc])
                src = pbf[:, 0:512].rearrange("p (c t) -> p c t", c=4)
                if i % 2 == 0:
                    op(ACT, lambda: se.copy(out=yfoxT[:, :, i * 128:(i + 1) * 128], in_=src), outs=[ByfT], ins=[BPB[bk]])
                else:
                    op(DVE, lambda: ve.tensor_copy(out=yfoxT[:, :, i * 128:(i + 1) * 128], in_=src), outs=[ByfT], ins=[BPB[bk]])
            K.barrier()
            K.pop()
            for nm_ in ("wq", "wk", "wv", "wff"):
                K.release(nm_)
            if stop == 2:
                raise _Stop()

        oT = K.sb("oT", [128, 4, S], BF16, persist=True)
        BoT = Buf("oT")
        if True:
            K.push()
            nwrow = K.sb("nwrow", [128, 512], F32)
            Bnw = Buf("nw")
            K.q_sp.dma(nwrow[:], nw_d.partition_broadcast(128), outs=[Bnw])
            whq = K.sb("whq", [128, 8, 128], BF16)
            whf = K.sb("whf", [128, 8, 128], BF16)
            whv = K.sb("whv", [128, 8, 256], BF16)
            Bwh = Buf("wh")
            T = [K.sb(f"T{i}", [128, S], F32) for i in range(5)]
            BT = [Buf(f"T{i}") for i in range(5)]
            qeT = K.sb("qeT", [128, S], BF16)
            keT = K.sb("keT", [128, S], BF16)
            klT = K.sb("klT", [128, S], BF16)
            Bqe, Bke, BklT = Buf("qe"), Buf("ke"), Buf("klT")
            kl = K.sb("kl", [128, NT, 128], BF16)
            Bkl = Buf("kl")
            vh = K.sb("vh", [128, NT, 128], BF16)
            Bvh = Buf("vh")
            nwsg = K.sb("nwsg", [128, NT, 128], F32)
            Bns = Buf("nwsg")
            sgt = [K.sb(f"sgt{i}", [128, 128], F32) for i in range(2)]
            Bsgt = [Buf(), Buf()]
            Dd = K.sb("Dd", [128, 32], F32)
            BDd = Buf("Dd")
            sf = [K.sb(f"state_f{i}", [128, 128], F32) for i in range(2)]
            Bsf = [Buf("sf0"), Buf("sf1")]
            U_sb = K.sb("U_sb", [128, 32, 128], F32)
            BU = [Buf(f"U{i}") for i in range(8)]
            stbf = K.sb("stbf", [128, 33, 128], BF16)
            Bsb = [Buf(f"stbf{c}") for c in range(33)]
            scm = [K.sb(f"scm{i}", [128, 128], BF16) for i in range(4)]
            Bscm = [Buf() for _ in range(4)]
            ofin = K.sb("ofin", [128, NT, 128], BF16)
            Bof = [Buf(f"of{i}") for i in range(NT)]
            ssq = K.sb("ssq", [128, NT, 2], F32)
            rstd = K.sb("rstd", [128, NT, 2], F32)
            junk = K.sb("junk", [128, 64], F32)
            Bss = [Buf(f"ssq{i}") for i in range(NT)]
            Bjk = Buf("junk")
            scn = [0]
            for pr in range(4):
                cs = pr * 128
                K.q_pl.dma(whq[:], win_r[:, :, O_HQ + cs:O_HQ + cs + 128], outs=[Bwh])
                K.q_pl.dma(whf[:], win_r[:, :, O_HF + cs:O_HF + cs + 128], outs=[Bwh])
                K.q_pl.dma(whv[:, :, 0:128], win_r[:, :, O_HI + cs:O_HI + cs + 128], outs=[Bwh])
                K.q_pl.dma(whv[:, :, 128:256], win_r[:, :, O_HG + cs:O_HG + cs + 128], outs=[Bwh])
                for tb in range(4):
                    sl = slice(tb * 512, (tb + 1) * 512)
                    bq = nbank(0, 4)
                    for k in range(8):
                        op(PEe, lambda: te.matmul(PB[bq][:, :], lhsT=whq[:, k, :], rhs=hT[:, k, sl], start=(k == 0), stop=(k == 7)),
                           outs=[BPB[bq]], ins=hT_blk(tb) + [Bwh])
                    bf_ = nbank(0, 4)
                    for k in range(8):
                        op(PEe, lambda: te.matmul(PB[bf_][:, :], lhsT=whf[:, k, :], rhs=hT[:, k, sl], start=(k == 0), stop=(k == 7)),
                           outs=[BPB[bf_]], ins=hT_blk(tb) + [Bwh])
                    op(ACT, lambda: se.activation(out=T[0][:, sl], in_=PB[bq][:, :], func=AF.Silu), outs=[BT[0]], ins=[BPB[bq]])
                    op(ACT, lambda: se.activation(out=T[1][:, sl], in_=PB[bf_][:, :], func=AF.Sigmoid), outs=[BT[1]], ins=[BPB[bf_]])
                op(ACT, lambda: se.activation(out=T[2][:], in_=T[1][:], func=AF.Ln, scale=omlb[:, pr:pr + 1], bias=lb[:, pr:pr + 1]),
                   outs=[BT[2]], ins=[BT[1], Bada])
                op(DVE, lambda: ve.tensor_scalar(out=T[3][:], in0=T[1][:], scalar1=nomlb[:, pr:pr + 1], scalar2=omlb[:, pr:pr + 1],
                                                 op0=ALU.mult, op1=ALU.add), outs=[BT[3]], ins=[BT[1], Bada])
                op(DVE, lambda: ve.tensor_tensor_scan(out=T[4][:], data0=rmask[:], data1=T[2][:], initial=0.0, op0=ALU.mult, op1=ALU.add),
                   outs=[BT[4]], ins=[BT[2], Bc])
                if pr == 0:
                    dump("bcum", T[4][:], [128, S], [BT[4]])
                op(ACT, lambda: se.activation(out=T[1][:], in_=T[4][:], func=AF.Exp), outs=[BT[1]], ins=[BT[4]])
                op(ACT, lambda: se.activation(out=T[2][:], in_=T[4][:], func=AF.Exp, scale=-1.0), outs=[BT[2]], ins=[BT[4]])
                op(DVE, lambda: ve.tensor_tensor(out=qeT[:], in0=T[0][:], in1=T[1][:], op=ALU.mult), outs=[Bqe], ins=[BT[0], BT[1]])
                op(POOL, lambda: ge.tensor_tensor(out=keT[:], in0=T[3][:], in1=T[2][:], op=ALU.mult), outs=[Bke], ins=[BT[3], BT[2]])
                op(DVE, lambda: ve.tensor_copy(out=Dd[:], in_=T[1][:, 63:S:64]), outs=[BDd], ins=[BT[1]])
                op(DVE, lambda: ve.tensor_tensor(out=T[0][:].rearrange("p (c t) -> p c t", t=64),
                                                 in0=T[4][:, 63:S:64].unsqueeze(2).to_broadcast([128, 32, 64]),
                                                 in1=T[4][:].rearrange("p (c t) -> p c t", t=64), op=ALU.subtract), outs=[BT[0]], ins=[BT[4]])
                op(ACT, lambda: se.activation(out=T[1][:], in_=T[0][:], func=AF.Exp), outs=[BT[1]], ins=[BT[0]])
                op(POOL, lambda: ge.tensor_tensor(out=klT[:], in0=T[3][:], in1=T[1][:], op=ALU.mult), outs=[BklT], ins=[BT[3], BT[1]])
                for g in range(4):
                    bk = nbank(0, 4)
                    pbf = PB[bk][:, :].bitcast(BF16)
                    for ii in range(4):
                        i = g * 4 + ii
                        op(PEe, lambda: te.transpose(pbf[:, ii * 128:(ii + 1) * 128], klT[:, i * 128:(i + 1) * 128], ident_b[:]),
                           outs=[BPB[bk]], ins=[BklT, Bc])
                    op(DVE, lambda: ve.tensor_copy(out=kl[:, g * 4:(g + 1) * 4, :], in_=pbf[:, 0:512].rearrange("p (i c) -> p i c", c=128)),
                       outs=[Bkl], ins=[BPB[bk]])
                for i in range(NT):
                    bk = nbank(0, 4)
                    for k in range(8):
                        op(PEe, lambda: te.matmul(PB[bk][:, 0:256], lhsT=hT[:, k, i * 128:(i + 1) * 128], rhs=whv[:, k, :], start=(k == 0), stop=(k == 7)),
                           outs=[BPB[bk]], ins=[BhT[i], Bwh])
                    op(ACT, lambda: se.copy(out=vh[:, i, :], in_=PB[bk][:, 0:128]), outs=[Bvh], ins=[BPB[bk]])
                    op(ACT, lambda: se.activation(out=sgt[i % 2][:], in_=PB[bk][:, 128:256], func=AF.Silu), outs=[Bsgt[i % 2]], ins=[BPB[bk]])
                    op(POOL, lambda: ge.tensor_tensor(out=nwsg[:, i, :], in0=sgt[i % 2][:], in1=nwrow[:, cs:cs + 128], op=ALU.mult),
                       outs=[Bns], ins=[Bsgt[i % 2], Bnw])
                U4 = U_sb[:].rearrange("p (t h) v -> p t h v", h=2)
                for g4 in range(4):
                    for half in range(2):
                        ub = 4 + 2 * (g4 % 2) + half
                        rows = slice(half * 64, half * 64 + 64)
                        for tt in range(4):
                            i = g4 * 4 + tt
                            op(PEe, lambda: te.matmul(PB[ub][:, tt * 128:(tt + 1) * 128], lhsT=kl[rows, i, :], rhs=vh[rows, i, :], start=True, stop=True),
                               outs=[BPB[ub]], ins=[Bkl, Bvh])
                    for half in range(2):
                        ub = 4 + 2 * (g4 % 2) + half
                        op(ACT, lambda: se.copy(out=U4[:, g4 * 4:(g4 + 1) * 4, half, :], in_=PB[ub][:, :].rearrange("p (c v) -> p c v", c=4)),
                           outs=[BU[g4 * 2], BU[g4 * 2 + 1]], ins=[BPB[ub]])
                op(POOL, lambda: ge.memset(sf[0][:], 0.0), outs=[Bsf[0]])
                op(POOL, lambda: ge.memset(sf[1][:], 0.0), outs=[Bsf[1]])
                op(POOL, lambda: ge.memset(stbf[:, 0, :], 0.0), outs=[Bsb[0]])
                for c in range(32):
                    src_, dst_ = sf[c % 2], sf[(c + 1) % 2]
                    for hh in range(2):
                        r = slice(hh * 64, hh * 64 + 64)
                        op(DVE, lambda: ve.scalar_tensor_tensor(out=dst_[r, r], in0=src_[r, r], scalar=Dd[r, c:c + 1],
                                                                in1=U_sb[r, c, hh * 64:hh * 64 + 64], op0=ALU.mult, op1=ALU.add),
                           outs=[Bsf[(c + 1) % 2]], ins=[Bsf[c % 2], BDd, BU[c // 4]])
                    op(ACT, lambda: se.copy(out=stbf[:, c + 1, :], in_=dst_[:, :]), outs=[Bsb[c + 1]], ins=[Bsf[(c + 1) % 2]])

                def o1(i):
                    tsl = slice(i * 128, (i + 1) * 128)
                    for hh in range(2):
                        r = slice(hh * 64, hh * 64 + 64)
                        bk = nbank(0, 4)
                        op(PEe, lambda: te.matmul(PB[bk][:, 0:128], lhsT=keT[r, tsl], rhs=qeT[r, tsl], start=True, stop=True),
                           outs=[BPB[bk]], ins=[Bke, Bqe])
                        si = (2 * i + hh) % 4
                        op(DVE, lambda: ve.tensor_tensor(out=scm[si][:], in0=PB[bk][:, 0:128], in1=hmask_f[:], op=ALU.mult),
                           outs=[Bscm[si]], ins=[BPB[bk], Bc])

                def o2(i):
                    ob = 6 + (i % 2)
                    for half in range(2):
                        c = 2 * i + half
                        rows = slice(half * 64, half * 64 + 64)
                        op(PEe, lambda: te.matmul(PB[ob][rows, 0:128], lhsT=qeT[:, c * 64:(c + 1) * 64], rhs=stbf[:, c, :], start=True, stop=False),
                           outs=[BPB[ob]], ins=[Bqe, Bsb[c]])
                    for hh in range(2):
                        si = (2 * i + hh) % 4
                        op(PEe, lambda: te.matmul(PB[ob][:, hh * 64:(hh + 1) * 64], lhsT=scm[si][:], rhs=vh[:, i, hh * 64:(hh + 1) * 64],
                                                  start=False, stop=(hh == 1)), outs=[BPB[ob]], ins=[Bscm[si], Bvh])
                    for hh in range(2):
                        op(ACT, lambda: se.activation(out=junk[:], in_=PB[ob][:, hh * 64:(hh + 1) * 64], func=AF.Square, accum_out=ssq[:, i, hh:hh + 1]),
                           outs=[Bss[i], Bjk], ins=[BPB[ob]])
                    op(DVE, lambda: ve.tensor_scalar(out=rstd[:, i, :], in0=ssq[:, i, :], scalar1=1.0 / 64.0, scalar2=RMS_EPS, op0=ALU.mult, op1=ALU.add),
                       outs=[Bss[i]], ins=[Bss[i]])
                    op(ACT, lambda: se.activation(out=rstd[:, i, :], in_=rstd[:, i, :], func=AF.Sqrt), outs=[Bss[i]], ins=[Bss[i]])
                    op(DVE, lambda: ve.reciprocal(out=rstd[:, i, :], in_=rstd[:, i, :]), outs=[Bss[i]], ins=[Bss[i]])
                    for hh in range(2):
                        op(DVE, lambda: ve.scalar_tensor_tensor(out=ofin[:, i, hh * 64:(hh + 1) * 64], in0=PB[ob][:, hh * 64:(hh + 1) * 64],
                                                                scalar=rstd[:, i, hh:hh + 1], in1=nwsg[:, i, hh * 64:(hh + 1) * 64],
                                                                op0=ALU.mult, op1=ALU.mult), outs=[Bof[i]], ins=[BPB[ob], Bss[i], Bns])

                o1(0)
                for i in range(NT):
                    if i + 1 < NT:
                        o1(i + 1)
                    o2(i)
                if pr == 0:
                    dump("ofin", ofin[:], [128, NT, 128], Bof, BF16)
                for g in range(4):
                    bk = nbank(0, 4)
                    pbf = PB[bk][:, :].bitcast(BF16)
                    for ii in range(4):
                        i = g * 4 + ii
                        op(PEe, lambda: te.transpose(pbf[:, ii * 128:(ii + 1) * 128], ofin[:, i, :], ident_b[:]), outs=[BPB[bk]], ins=[Bof[i], Bc])
                    op(ACT, lambda: se.copy(out=oT[:, pr, g * 512:(g + 1) * 512], in_=pbf[:, 0:512]), outs=[BoT], ins=[BPB[bk]])
            K.barrier()
            K.pop()
            if stop == 3:
                raise _Stop()

        mergedT = K.sb("mergedT", [128, 8, S], BF16, persist=True)
        BmT = [Buf(f"mT{i}") for i in range(4)]
        if True:
            K.push()
            wupf = K.sb("wupf", [128, 4, D], BF16)
            wuph = K.sb("wuph", [128, 4, D], BF16)
            wgf = K.sb("wgf", [128, 8, D], BF16)
            wgh = K.sb("wgh", [128, 8, D], BF16)
            Bwc = Buf("wC")
            K.q_pl.dma(wupf[:], wupf_d.rearrange("(k p) n -> p k n", p=128), outs=[Bwc])
            K.q_pl.dma(wuph[:], wuph_d.rearrange("(k p) n -> p k n", p=128), outs=[Bwc])
            for g in range(2):
                K.q_pl.dma(wgf[:, :, g * 512:(g + 1) * 512], win_r[:, :, O_GF + g * 512:O_GF + (g + 1) * 512], outs=[Bwc])
                K.q_pl.dma(wgh[:, :, g * 512:(g + 1) * 512], win_r[:, :, O_GH + g * 512:O_GH + (g + 1) * 512], outs=[Bwc])
            tmp = [[K.sb(f"mt{a}{i}", [128, 512], F32) for i in range(2)] for a in range(4)]
            Btmp = [[Buf() for i in range(2)] for a in range(4)]
            n = 0
            for dc in range(8):
                dsl = slice(dc * 128, (dc + 1) * 128)
                for tb in range(4):
                    sl = slice(tb * 512, (tb + 1) * 512)
                    p = n % 2
                    n += 1
                    b_uf, b_uh, b_gf, b_gh = 4 * p, 4 * p + 1, 4 * p + 2, 4 * p + 3
                    for k in range(8):
                        op(PEe, lambda: te.matmul(PB[b_gf][:, :], lhsT=wgf[:, k, dsl], rhs=hT[:, k, sl], start=(k == 0), stop=(k == 7)),
                           outs=[BPB[b_gf]], ins=hT_blk(tb) + [Bwc])
                    for k in range(8):
                        op(PEe, lambda: te.matmul(PB[b_gh][:, :], lhsT=wgh[:, k, dsl], rhs=hT[:, k, sl], start=(k == 0), stop=(k == 7)),
                           outs=[BPB[b_gh]], ins=hT_blk(tb) + [Bwc])
                    for k in range(4):
                        op(PEe, lambda: te.matmul(PB[b_uf][:, :], lhsT=wupf[:, k, dsl], rhs=yfoxT[:, k, sl], start=(k == 0), stop=(k == 3)),
                           outs=[BPB[b_uf]], ins=[ByfT, Bwc])
                    for k in range(4):
                        op(PEe, lambda: te.matmul(PB[b_uh][:, :], lhsT=wuph[:, k, dsl], rhs=oT[:, k, sl], start=(k == 0), stop=(k == 3)),
                           outs=[BPB[b_uh]], ins=[BoT, Bwc])
                    op(ACT, lambda: se.activation(out=tmp[0][p][:], in_=PB[b_gf][:, :], func=AF.Sigmoid), outs=[Btmp[0][p]], ins=[BPB[b_gf]])
                    op(ACT, lambda: se.activation(out=tmp[1][p][:], in_=PB[b_gh][:, :], func=AF.Sigmoid), outs=[Btmp[1][p]], ins=[BPB[b_gh]])
                    op(DVE, lambda: ve.tensor_tensor(out=tmp[2][p][:], in0=PB[b_uf][:, :], in1=tmp[0][p][:], op=ALU.mult),
                       outs=[Btmp[2][p]], ins=[BPB[b_uf], Btmp[0][p]])
                    op(DVE, lambda: ve.tensor_tensor(out=tmp[3][p][:], in0=PB[b_uh][:, :], in1=tmp[1][p][:], op=ALU.mult),
                       outs=[Btmp[3][p]], ins=[BPB[b_uh], Btmp[1][p]])
                    op(POOL, lambda: ge.tensor_tensor(out=mergedT[:, dc, sl], in0=tmp[2][p][:], in1=tmp[3][p][:], op=ALU.add),
                       outs=[BmT[tb]], ins=[Btmp[2][p], Btmp[3][p]])
            dump("mergedT", mergedT[:], [128, 8, S], BmT, BF16)
            K.barrier()
            K.pop()
            if stop == 4:
                raise _Stop()
            K.release("hT")
            K.release("yfoxT")
            K.release("oT")

        G2row = K.sb("G2row", [128, D], F32)
        L2G = K.sb("L2G", [128, D], F32)
        L2B = K.sb("L2B", [128, D], F32)
        Brow2 = Buf("rows2")
        K.q_sp.dma(L2G[:], ln2g_d.partition_broadcast(128), outs=[Brow2])
        K.q_sp.dma(L2B[:], ln2b_d.partition_broadcast(128), outs=[Brow2])

        h2T = K.sb("h2T", [128, 8, S], BF16, persist=True)
        h2tok = K.sb("h2tok", [128, NT, D], BF16, persist=True)
        Bh2k = [Buf(f"h2k{i}") for i in range(NT)]
        Bh2T = [Buf(f"h2T{i}") for i in range(NT)]
        Bx1d = [Buf(f"x1d{i}") for i in range(NT)]
        if True:
            K.push()
            G1row = K.sb("G1row", [128, D], F32)
            L1G = K.sb("L1G", [128, D], F32)
            L1B = K.sb("L1B", [128, D], F32)
            A2 = K.sb("A2", [128, D], F32)
            B2 = K.sb("B2", [128, D], F32)
            Brow = Buf("rows1")
            K.q_sp.dma(L1G[:], ln1g_d.partition_broadcast(128), outs=[Brow])
            K.q_sp.dma(L1B[:], ln1b_d.partition_broadcast(128), outs=[Brow])
            K.push()
            bcast_rows(G2row, Brow2, adaT[:, 40:48])
            bcast_rows(G1row, Brow, adaT[:, 16:24])
            bcast_rows(A2, Brow, sc2p)
            bcast_rows(B2, Brow, adaT[:, 24:32])
            tmpr = K.sb("tmpr", [128, D], F32)
            Btr = Buf()
            op(DVE, lambda: ve.tensor_tensor(out=tmpr[:], in0=L1B[:], in1=A2[:], op=ALU.mult), outs=[Btr], ins=[Brow])
            op(DVE, lambda: ve.tensor_tensor(out=B2[:], in0=B2[:], in1=tmpr[:], op=ALU.add), outs=[Brow], ins=[Brow, Btr])
            op(DVE, lambda: ve.tensor_tensor(out=A2[:], in0=A2[:], in1=L1G[:], op=ALU.mult), outs=[Brow], ins=[Brow])
            K.barrier()
            K.pop()
            if stop == 5:
                raise _Stop()
            wout = K.sb("wout", [128, 8, D], BF16)
            Bwo = Buf("wout")
            K.q_pl.dma(wout[:], wout_d.rearrange("(k p) n -> p k n", p=128), outs=[Bwo])
            xin = [K.sb(f"xin2{i}", [128, D], F32) for i in range(3)]
            Bxin = [Buf(), Buf(), Buf()]
            for i in range(2):
                K.q_sp.dma(xin[i][:], x_d[i * 128:(i + 1) * 128, :], outs=[Bxin[i]])
            z = [K.sb(f"z{i}", [128, D], F32) for i in range(2)]
            Bz = [Buf(), Buf()]
            zn = [K.sb(f"zn{i}", [128, D], F32) for i in range(2)]
            Bzn = [Buf(), Buf()]
            x1t = [K.sb(f"x1t{i}", [128, D], F32) for i in range(2)]
            Bx1t = [Buf(), Buf()]
            h2f = [K.sb(f"h2f{i}", [128, D], F32) for i in range(2)]
            Bh2f = [Buf(), Buf()]
            st = K.sb("bnst", [128, 2, 6], F32)
            mvA = K.sb("bnmv", [128, NT, 2], F32)
            rsA = K.sb("lnrs", [128, NT, 2], F32)
            Bstt = Buf("bnst")
            Bst_t = [Buf(f"st{i}") for i in range(NT)]
            def dA(i):
                p = i % 2
                tsl = slice(i * 128, (i + 1) * 128)
                yb = [4 * p, 4 * p + 1]
                for half in range(2):
                    for k in range(8):
                        op(PEe, lambda: te.matmul(PB[yb[half]][:, :], lhsT=mergedT[:, k, tsl], rhs=wout[:, k, half * 512:(half + 1) * 512],
                                                  start=(k == 0), stop=(k == 7)), outs=[BPB[yb[half]]], ins=[BmT[i // 4], Bwo])

            def dB(i):
                p = i % 2
                px = i % 3
                if i + 2 < NT:
                    K.q_sp.dma(xin[(i + 2) % 3][:], x_d[(i + 2) * 128:(i + 3) * 128, :], outs=[Bxin[(i + 2) % 3]])
                yb = [4 * p, 4 * p + 1]
                for half in range(2):
                    hs = slice(half * 512, (half + 1) * 512)
                    op(DVE, lambda: ve.tensor_tensor(out=z[p][:, hs], in0=PB[yb[half]][:, :], in1=G1row[:, hs], op=ALU.mult),
                       outs=[Bz[p]], ins=[BPB[yb[half]], Brow])
                op(DVE, lambda: ve.scalar_tensor_tensor(out=z[p][:], in0=xin[px][:], scalar=ALPHA, in1=z[p][:], op0=ALU.mult, op1=ALU.add),
                   outs=[Bz[p]], ins=[Bxin[px], Bz[p]])
                mv = mvA[:, i, :]
                rs = rsA[:, i, 0:1]
                nmr = rsA[:, i, 1:2]
                for half in range(2):
                    op(DVE, lambda: ve.bn_stats(out=st[:, half, :], in_=z[p][:, half * 512:(half + 1) * 512]), outs=[Bstt], ins=[Bz[p]])
                op(DVE, lambda: ve.bn_aggr(out=mv, in_=st[:].rearrange("p a b -> p (a b)")), outs=[Bst_t[i]], ins=[Bstt])
                op(DVE, lambda: ve.tensor_scalar(out=rs, in0=mv[:, 1:2], scalar1=LN_EPS, scalar2=None, op0=ALU.add), outs=[Bst_t[i]], ins=[Bst_t[i]])
                op(ACT, lambda: se.activation(out=rs, in_=rs, func=AF.Sqrt), outs=[Bst_t[i]], ins=[Bst_t[i]])
                op(DVE, lambda: ve.reciprocal(out=rs, in_=rs), outs=[Bst_t[i]], ins=[Bst_t[i]])
                op(DVE, lambda: ve.tensor_scalar(out=nmr, in0=mv[:, 0:1], scalar1=rs, scalar2=-1.0, op0=ALU.mult, op1=ALU.mult),
                   outs=[Bst_t[i]], ins=[Bst_t[i]])
                op(ACT, lambda: se.activation(out=zn[p][:], in_=z[p][:], func=AF.Identity, scale=rs, bias=nmr), outs=[Bzn[p]], ins=[Bz[p], Bst_t[i]])

            def dC(i):
                p = i % 2
                tsl = slice(i * 128, (i + 1) * 128)
                op(DVE, lambda: ve.tensor_tensor(out=h2f[p][:], in0=zn[p][:], in1=A2[:], op=ALU.mult), outs=[Bh2f[p]], ins=[Bzn[p], Brow])
                op(POOL, lambda: ge.tensor_tensor(out=h2tok[:, i, :], in0=h2f[p][:], in1=B2[:], op=ALU.add), outs=[Bh2k[i]], ins=[Bh2f[p], Brow])
                op(POOL, lambda: ge.tensor_tensor(out=x1t[p][:], in0=zn[p][:], in1=L1G[:], op=ALU.mult), outs=[Bx1t[p]], ins=[Bzn[p], Brow])
                op(POOL, lambda: ge.tensor_tensor(out=x1t[p][:], in0=x1t[p][:], in1=L1B[:], op=ALU.add), outs=[Bx1t[p]], ins=[Bx1t[p], Brow])
                K.q_sp.dma(x1_d[tsl, :], x1t[p][:], outs=[Bx1d[i]], ins=[Bx1t[p]])

            def dD(i):
                p = i % 2
                tsl = slice(i * 128, (i + 1) * 128)
                tb_ = 2 + 4 * p
                pbf = PB[tb_][:, :].bitcast(BF16)
                for c in range(8):
                    op(PEe, lambda: te.transpose(pbf[:, c * 128:(c + 1) * 128], h2tok[:, i, c * 128:(c + 1) * 128], ident_b[:]),
                       outs=[BPB[tb_]], ins=[Bh2k[i], Bc])
                op(ACT, lambda: se.copy(out=h2T[:, :, tsl], in_=pbf[:, :].rearrange("p (c t) -> p c t", c=8)), outs=[Bh2T[i]], ins=[BPB[tb_]])

            dA(0)
            dA(1)
            dB(0)
            for i in range(NT):
                if i + 1 < NT:
                    dB(i + 1)
                dC(i)
                if i + 2 < NT:
                    dA(i + 2)
                dD(i)
            dump("h2T", h2T[:], [128, 8, S], Bh2T, BF16)
            K.barrier()
            K.pop()
            if stop == 6:
                raise _Stop()
            K.release("mergedT")

        NS = 96
        I32 = mybir.dt.int32
        hs_d = nc.dram_tensor("hs_scratch", [NS * 128, D], BF16, kind="Internal").ap()
        ys_d = nc.dram_tensor("ys_scratch", [NS * 128, D], F32, kind="Internal").ap()
        Bhs = Buf("hs_d")
        Bys = [Buf(f"ys{s_}") for s_ in range(NS)]

        _breg = {}

        def idma(out, out_off, in_, in_off, outs, ins, bound=None):
            bval = NS * 128 - 1 if bound is None else bound
            if bval not in _breg:
                _breg[bval] = ge.to_reg(bval)
            I = K.pool
            for b_ in ins:
                I.wait(b_.w)
                for t in b_.wl:
                    I.wait(t)
            for b_ in outs:
                I.wait(b_.w)
                for t in b_.wl:
                    I.wait(t)
                for t in b_.r.values():
                    I.wait(t)
                for t in b_.rd:
                    I.wait(t)
            q = K.q_pl
            j = q.rr
            q.rr = (q.rr + 1) % len(q.sems)
            key = f"{q.name}{j}"
            if q.vals[j] > 0:
                I.wait(DTok(q.sems[j], q.vals[j], key))
            ins_ = ge.indirect_dma_start(out=out, out_offset=out_off, in_=in_, in_offset=in_off,
                                             bounds_check=_breg[bval], oob_is_err=False)
            ins_.then_inc(q.sems[j], 16)
            q.vals[j] += 16
            tok = DTok(q.sems[j], q.vals[j], key)
            for b_ in ins:
                b_.rd.append(tok)
            for b_ in outs:
                if isinstance(b_.w, DTok):
                    b_.wl.append(b_.w)
                b_.w = tok
                b_.r = {}
                b_.rd = []
            return tok

        if True:
            K.push()
            wr = K.sb("wr", [128, 8, 72], BF16)
            brow = K.sb("brow", [128, 72], F32)
            Bwr = Buf("wr")
            K.q_pl.dma(wr[:], wr_d.rearrange("(k p) n -> p k n", p=128), outs=[Bwr])
            K.q_sp.dma(brow[:], br_d.partition_broadcast(128), outs=[Bwr])
            MK = K.sb("MK", [128, 2, NT, 64], F32)
            W12 = K.sb("W12", [128, NT, 2], F32)
            Lall = K.sb("Lall", [128, NT, 72], F32)
            lgs = K.sb("lgs", [128, NT, 8], F32)
            gmk = K.sb("gmk", [128, NT, 8], F32)
            lem = K.sb("lem", [128, NT, 64], F32)
            lem2 = K.sb("lem2", [128, NT, 64], F32)
            sm = K.sb("sm", [128, 8, NT], F32)
            Br = Buf("router")
            RB = [0, 1, 2]
            for i in range(NT):
                tsl = slice(i * 128, (i + 1) * 128)
                bk = RB[i // 7]
                cs_ = (i % 7) * 72
                for k in range(8):
                    op(PEe, lambda: te.matmul(PB[bk][:, cs_:cs_ + 72], lhsT=h2T[:, k, tsl], rhs=wr[:, k, :], start=(k == 0), stop=(k == 7)),
                       outs=[BPB[bk]], ins=[Bh2T[i], Bwr])
            R = lambda fn, eng=DVE, extra=(): op(eng, fn, outs=[Br], ins=[Br] + list(extra))
            for g in range(3):
                n_ = min(7, NT - 7 * g)
                R(lambda: ve.tensor_tensor(out=Lall[:, 7 * g:7 * g + n_, :], in0=PB[RB[g]][:, 0:n_ * 72].rearrange("p (i c) -> p i c", c=72),
                                           in1=brow[:].unsqueeze(1).to_broadcast([128, n_, 72]), op=ALU.add), extra=[BPB[RB[g]], Bwr])
            gmax, gsum, g_w, m1, m2, dd, w1 = [sm[:, j, :] for j in range(7)]
            bc8 = lambda a_: a_.unsqueeze(2).to_broadcast([128, NT, 8])
            bc64 = lambda a_: a_.unsqueeze(2).to_broadcast([128, NT, 64])
            R(lambda: ve.tensor_reduce(out=gmax, in_=Lall[:, :, 0:8], axis=AX.X, op=ALU.max))
            R(lambda: ve.tensor_tensor(out=lgs[:], in0=Lall[:, :, 0:8], in1=bc8(gmax), op=ALU.subtract))
            R(lambda: se.activation(out=gmk[:], in_=lgs[:], func=AF.Exp), eng=ACT)
            R(lambda: ve.tensor_reduce(out=gsum, in_=gmk[:], axis=AX.X, op=ALU.add))
            R(lambda: ve.reciprocal(out=g_w, in_=gsum))
            R(lambda: ve.tensor_scalar(out=gmk[:], in0=lgs[:], scalar1=0.0, scalar2=None, op0=ALU.is_ge))
            R(lambda: ve.tensor_scalar(out=gmk[:], in0=gmk[:], scalar1=1.0, scalar2=BIG, op0=ALU.subtract, op1=ALU.mult))
            R(lambda: ve.tensor_tensor(out=lem[:].rearrange("p i (g j) -> p i g j", j=8), in0=Lall[:, :, 8:72].rearrange("p i (g j) -> p i g j", j=8),
                                       in1=gmk[:].unsqueeze(3).to_broadcast([128, NT, 8, 8]), op=ALU.add))
            R(lambda: ve.tensor_reduce(out=m1, in_=lem[:], axis=AX.X, op=ALU.max))
            R(lambda: ve.tensor_tensor(out=MK[:, 0, :, :], in0=lem[:], in1=bc64(m1), op=ALU.is_ge))
            R(lambda: ve.scalar_tensor_tensor(out=lem2[:].rearrange("p i e -> p (i e)"), in0=MK[:, 0, :, :].rearrange("p i e -> p (i e)"), scalar=-BIG,
                                              in1=lem[:].rearrange("p i e -> p (i e)"), op0=ALU.mult, op1=ALU.add))
            R(lambda: ve.tensor_reduce(out=m2, in_=lem2[:], axis=AX.X, op=ALU.max))
            R(lambda: ve.tensor_tensor(out=MK[:, 1, :, :], in0=lem2[:], in1=bc64(m2), op=ALU.is_ge))
            R(lambda: ve.tensor_tensor(out=dd, in0=m1, in1=m2, op=ALU.subtract))
            R(lambda: se.activation(out=w1, in_=dd, func=AF.Sigmoid), eng=ACT)
            R(lambda: ve.tensor_tensor(out=W12[:, :, 0], in0=w1, in1=g_w, op=ALU.mult))
            R(lambda: ve.tensor_tensor(out=W12[:, :, 1], in0=g_w, in1=W12[:, :, 0], op=ALU.subtract))
            K.barrier()
            K.release("h2T")
            dump("MK", MK[:], [128, 2, NT, 64], [Br])
            dump("W12", W12[:], [128, NT, 2], [Br])

            Ab = K.sb("Ab", [128, NT * 64], BF16)
            stri_b = K.sb("stri_b", [128, 128], BF16)
            ones_b = K.sb("ones_b", [128, 128], BF16)
            POS = K.sb("POS", [128, NT, 64], F32)
            TOT = K.sb("TOT", [128, NT, 64], F32)
            CAR = K.sb("CAR", [128, NT, 64], F32)
            TM = K.sb("TMd", [128, NT, 64], F32)
            cnt = K.sb("cnt", [128, 64], F32)
            pad = K.sb("pad", [128, 64], F32)
            cend = K.sb("cend", [128, 64], F32)
            base = K.sb("base", [128, 64], F32)
            one64 = K.sb("one64", [128, 64], F32)
            Pf = K.sb("Pf", [128, 2, NT], F32)
            Pi = K.sb("Pi", [128, 2, NT], I32)
            Bd = Buf("dispatch")
            Dd_ = lambda fn, eng=DVE, extra=(): op(eng, fn, outs=[Bd], ins=[Bd] + list(extra))
            Dd_(lambda: ge.memset(stri_b[:], 1.0), eng=POOL)
            Dd_(lambda: ge.affine_select(out=stri_b[:], in_=stri_b[:], compare_op=ALU.is_gt, fill=0.0, base=0, pattern=[[1, 128]],
                                         channel_multiplier=-1), eng=POOL)
            Dd_(lambda: ge.memset(ones_b[:], 1.0), eng=POOL)
            Dd_(lambda: ge.memset(one64[:], 1.0), eng=POOL)
            Dd_(lambda: ve.tensor_tensor(out=Ab[:], in0=MK[:, 0, :, :].rearrange("p i e -> p (i e)"), in1=MK[:, 1, :, :].rearrange("p i e -> p (i e)"),
                                         op=ALU.add), extra=[Br])
            for half in range(2):
                b1, b2 = nbank(0, 8), nbank(0, 8)
                hsl_ = slice(half * 512, (half + 1) * 512)
                op(PEe, lambda: te.matmul(PB[b1][:, :], lhsT=stri_b[:], rhs=Ab[:, hsl_], start=True, stop=True), outs=[BPB[b1]], ins=[Bd])
                op(PEe, lambda: te.matmul(PB[b2][:, :], lhsT=ones_b[:], rhs=Ab[:, hsl_], start=True, stop=True), outs=[BPB[b2]], ins=[Bd])
                Dd_(lambda: ve.tensor_copy(out=POS[:].rearrange("p i e -> p (i e)")[:, hsl_], in_=PB[b1][:, :]), extra=[BPB[b1]])
                Dd_(lambda: ve.tensor_copy(out=TOT[:].rearrange("p i e -> p (i e)")[:, hsl_], in_=PB[b2][:, :]), extra=[BPB[b2]])
            Dd_(lambda: ve.memset(CAR[:, 0, :], 0.0))
            for i in range(1, NT):
                Dd_(lambda: ve.tensor_tensor(out=CAR[:, i, :], in0=CAR[:, i - 1, :], in1=TOT[:, i - 1, :], op=ALU.add))
            Dd_(lambda: ve.tensor_tensor(out=cnt[:], in0=CAR[:, NT - 1, :], in1=TOT[:, NT - 1, :], op=ALU.add))
            Dd_(lambda: ve.memset(pad[:], 0.0))
            for k in range(16):
                Dd_(lambda: ve.scalar_tensor_tensor(out=pad[:], in0=cnt[:], scalar=128.0 * k, in1=pad[:], op0=ALU.is_gt, op1=ALU.add))
            Dd_(lambda: ve.tensor_scalar(out=pad[:], in0=pad[:], scalar1=128.0, scalar2=None, op0=ALU.mult))
            Dd_(lambda: ve.tensor_tensor_scan(out=cend[:], data0=one64[:], data1=pad[:], initial=0.0, op0=ALU.mult, op1=ALU.add))
            Dd_(lambda: ve.tensor_tensor(out=base[:], in0=cend[:], in1=pad[:], op=ALU.subtract))
            Dd_(lambda: ve.tensor_tensor(out=CAR[:], in0=CAR[:], in1=base[:].unsqueeze(1).to_broadcast([128, NT, 64]), op=ALU.add))
            Dd_(lambda: ve.tensor_tensor(out=POS[:], in0=POS[:], in1=CAR[:], op=ALU.add))
            for j in range(2):
                Dd_(lambda: ve.tensor_tensor(out=TM[:], in0=MK[:, j, :, :], in1=POS[:], op=ALU.mult), extra=[Br])
                Dd_(lambda: ve.tensor_reduce(out=Pf[:, j, :], in_=TM[:], axis=AX.X, op=ALU.add))
            Dd_(lambda: ve.tensor_copy(out=Pi[:], in_=Pf[:]))
            dump("Pf", Pf[:], [128, 2, NT], [Bd])
            dump("cend", cend[:], [128, 64], [Bd])
            sl_i = K.sb("sl_i", [128, NS], I32)
            slf = K.sb("slf", [128, NS], F32)
            pcol_i = K.sb("pcol_i", [128, 1], I32)
            pcol = K.sb("pcol", [128, 1], F32)
            Erow = K.sb("Erow", [128, NS], F32)
            idxW = K.sb("idxW", [128, NS], I32)
            Dd_(lambda: ge.iota(out=sl_i[:], pattern=[[128, NS]], base=0, channel_multiplier=0), eng=POOL)
            Dd_(lambda: ge.iota(out=pcol_i[:], pattern=[[0, 1]], base=0, channel_multiplier=1), eng=POOL)
            Dd_(lambda: ve.tensor_copy(out=slf[:], in_=sl_i[:]))
            Dd_(lambda: ve.tensor_copy(out=pcol[:], in_=pcol_i[:]))
            K.push()
            cmp_ = K.sb("cmp_", [128, 32, 64], F32)
            for c3 in range(NS // 32):
                csl = slice(c3 * 32, (c3 + 1) * 32)
                Dd_(lambda: ve.tensor_tensor(out=cmp_[:], in0=cend[:].unsqueeze(1).to_broadcast([128, 32, 64]),
                                             in1=slf[:, csl].unsqueeze(2).to_broadcast([128, 32, 64]), op=ALU.is_le))
                Dd_(lambda: ve.tensor_reduce(out=Erow[:, csl], in_=cmp_[:], axis=AX.X, op=ALU.add))
            Dd_(lambda: ve.tensor_scalar(out=Erow[:], in0=Erow[:], scalar1=128.0, scalar2=pcol[:, 0:1], op0=ALU.mult, op1=ALU.add))
            Dd_(lambda: ve.tensor_copy(out=idxW[:], in_=Erow[:]))
            K.barrier()
            K.pop()
            dump("idxW", idxW[:], [128, NS], [Bd], I32)

            Bhs_l = [Buf(f"hs{i}") for i in range(2 * NT)]
            for i in range(NT):
                for j in range(2):
                    idma(hs_d[:, :], bass.IndirectOffsetOnAxis(ap=Pi[:, j, i:i + 1], axis=0), h2tok[:, i, :], None,
                         outs=[Bhs_l[2 * i + j]], ins=[Bh2k[i], Bd])
            if stop == 6.5:
                K.barrier()
                raise _Stop()

            NWB = 6
            K.push()
            wg = [K.sb(f"wg{i}", [128, 8, 256], BF16) for i in range(NWB)]
            wu = [K.sb(f"wu{i}", [128, 8, 256], BF16) for i in range(NWB)]
            wd = [K.sb(f"wd{i}", [128, 2, D], BF16) for i in range(NWB)]
            Bwg = [Buf() for _ in range(NWB)]
            Bwu = [Buf() for _ in range(NWB)]
            Bwd = [Buf() for _ in range(NWB)]
            NHB = 4
            hsl = [K.sb(f"hsl{i}", [128, D], BF16) for i in range(NHB)]
            Bhsl = [Buf() for _ in range(NHB)]
            hsT = [K.sb(f"hsT{i}", [128, 8, 128], BF16) for i in range(2)]
            BhsT = [Buf(), Buf()]
            sgs = [K.sb(f"sgs{i}", [128, 256], F32) for i in range(2)]
            Bsgs = [Buf(), Buf()]
            hid = [K.sb(f"hid{i}", [128, 2, 128], BF16) for i in range(2)]
            Bhid = [Buf(), Buf()]
            ysl = [K.sb(f"ysl{i}", [128, D], F32) for i in range(2)]
            Bysl = [Buf(), Buf()]
            def load_w(s_):
                w_ = s_ % NWB
                off = bass.IndirectOffsetOnAxis(ap=idxW[:, s_:s_ + 1], axis=0)
                idma(wg[w_][:].rearrange("p k f -> p (k f)"), None, weg_d[:, :], off, outs=[Bwg[w_]], ins=[Bd], bound=NEXP * 128 - 1)
                idma(wu[w_][:].rearrange("p k f -> p (k f)"), None, weu_d[:, :], off, outs=[Bwu[w_]], ins=[Bd], bound=NEXP * 128 - 1)
                idma(wd[w_][:].rearrange("p k f -> p (k f)"), None, wed_d[:, :], off, outs=[Bwd[w_]], ins=[Bd], bound=NEXP * 128 - 1)

            def stageL(s_):
                h_ = s_ % NHB
                K.q_sp.dma(hsl[h_][:], hs_d[s_ * 128:(s_ + 1) * 128, :], outs=[Bhsl[h_]], ins=Bhs_l)

            def stageA(s_):
                p = s_ % 2
                h_ = s_ % NHB
                tb_ = p
                pbf = PB[tb_][:, :].bitcast(BF16)
                for c in range(8):
                    op(PEe, lambda: te.transpose(pbf[:, c * 128:(c + 1) * 128], hsl[h_][:, c * 128:(c + 1) * 128], ident_b[:]),
                       outs=[BPB[tb_]], ins=[Bhsl[h_], Bc])
                op(ACT, lambda: se.copy(out=hsT[p][:], in_=pbf[:, :].rearrange("p (c t) -> p c t", c=8)), outs=[BhsT[p]], ins=[BPB[tb_]])

            def stageB(s_):
                p = s_ % 2
                w_ = s_ % NWB
                bg, bu = 2 + p, 4 + p
                for fc in range(2):
                    fsl = slice(fc * 128, (fc + 1) * 128)
                    for k in range(8):
                        op(PEe, lambda: te.matmul(PB[bg][:, fsl], lhsT=wg[w_][:, k, fsl], rhs=hsT[p][:, k, :], start=(k == 0), stop=(k == 7)),
                           outs=[BPB[bg]], ins=[Bwg[w_], BhsT[p]])
                for fc in range(2):
                    fsl = slice(fc * 128, (fc + 1) * 128)
                    for k in range(8):
                        op(PEe, lambda: te.matmul(PB[bu][:, fsl], lhsT=wu[w_][:, k, fsl], rhs=hsT[p][:, k, :], start=(k == 0), stop=(k == 7)),
                           outs=[BPB[bu]], ins=[Bwu[w_], BhsT[p]])
                op(ACT, lambda: se.activation(out=sgs[p][:], in_=PB[bg][:, 0:256], func=AF.Silu), outs=[Bsgs[p]], ins=[BPB[bg]])
                op(DVE, lambda: ve.tensor_tensor(out=hid[p][:].rearrange("p a t -> p (a t)"), in0=PB[bu][:, 0:256], in1=sgs[p][:], op=ALU.mult),
                   outs=[Bhid[p]], ins=[BPB[bu], Bsgs[p]])

            def stageC(s_):
                p = s_ % 2
                w_ = s_ % NWB
                for half in range(2):
                    yb = 6 + half
                    for fc in range(2):
                        op(PEe, lambda: te.matmul(PB[yb][:, :], lhsT=hid[p][:, fc, :], rhs=wd[w_][:, fc, half * 512:(half + 1) * 512],
                                                  start=(fc == 0), stop=(fc == 1)), outs=[BPB[yb]], ins=[Bhid[p], Bwd[w_]])
                op(ACT, lambda: se.copy(out=ysl[p][:, 0:512], in_=PB[6][:, :]), outs=[Bysl[p]], ins=[BPB[6]])
                op(DVE, lambda: ve.tensor_copy(out=ysl[p][:, 512:1024], in_=PB[7][:, :]), outs=[Bysl[p]], ins=[BPB[7]])
                K.q_sp.dma(ys_d[s_ * 128:(s_ + 1) * 128, :], ysl[p][:], outs=[Bys[s_]], ins=[Bysl[p]])

            for s_ in range(min(NWB - 1, NS)):
                load_w(s_)
            for s_ in range(min(NHB - 1, NS)):
                stageL(s_)
            stageA(0)
            for s_ in range(NS):
                if s_ + NHB - 1 < NS:
                    stageL(s_ + NHB - 1)
                if s_ + 1 < NS:
                    stageA(s_ + 1)
                stageB(s_)
                if s_ >= 1:
                    stageC(s_ - 1)
                if s_ + NWB - 1 < NS:
                    load_w(s_ + NWB - 1)
            stageC(NS - 1)
            K.barrier()
            K.pop()
            K.release("h2tok")
            if stop == 7:
                raise _Stop()

            NFB = 3
            xin = [K.sb(f"x1in{i}", [128, D], F32) for i in range(NFB)]
            Bxin = [Buf() for _ in range(NFB)]
            g1 = [K.sb(f"g1_{i}", [128, D], F32) for i in range(NFB)]
            g2 = [K.sb(f"g2_{i}", [128, D], F32) for i in range(NFB)]
            Bg1 = [Buf() for _ in range(NFB)]
            Bg2 = [Buf() for _ in range(NFB)]

            def fetch(i):
                q_ = i % NFB
                K.q_sp.dma(xin[q_][:], x1_d[i * 128:(i + 1) * 128, :], outs=[Bxin[q_]], ins=[Bx1d[i]])
                idma(g1[q_][:, :], None, ys_d[:, :], bass.IndirectOffsetOnAxis(ap=Pi[:, 0, i:i + 1], axis=0), outs=[Bg1[q_]], ins=Bys + [Bd])
                idma(g2[q_][:, :], None, ys_d[:, :], bass.IndirectOffsetOnAxis(ap=Pi[:, 1, i:i + 1], axis=0), outs=[Bg2[q_]], ins=Bys + [Bd])

            for i in range(NFB - 1):
                fetch(i)
            z = [K.sb(f"z2{i}", [128, D], F32) for i in range(3)]
            Bz = [Buf(), Buf(), Buf()]
            ot = [K.sb(f"ot{i}", [128, D], F32) for i in range(2)]
            Bot = [Buf(), Buf()]
            st = K.sb("bnst2", [128, 2, 6], F32)
            mvA = K.sb("bnmv2", [128, NT, 2], F32)
            rsA = K.sb("lnrs2", [128, NT, 2], F32)
            Bstt = Buf("bnst2")
            Bst_t = [Buf(f"st2_{i}") for i in range(NT)]
            outs_t = []
            def f1(i):
                p = i % 2
                q_ = i % NFB
                op(DVE, lambda: ve.tensor_scalar(out=g1[q_][:], in0=g1[q_][:], scalar1=W12[:, i, 0:1], scalar2=None, op0=ALU.mult),
                   outs=[Bg1[q_]], ins=[Bg1[q_], Br])
                op(DVE, lambda: ve.scalar_tensor_tensor(out=g1[q_][:], in0=g2[q_][:], scalar=W12[:, i, 1:2], in1=g1[q_][:], op0=ALU.mult, op1=ALU.add),
                   outs=[Bg1[q_]], ins=[Bg1[q_], Bg2[q_], Br])
                if i == 0:
                    dump("y0", g1[q_][:], [128, D], [Bg1[q_]])
                op(DVE, lambda: ve.tensor_tensor(out=z[i % 3][:], in0=g1[q_][:], in1=G2row[:], op=ALU.mult), outs=[Bz[i % 3]], ins=[Bg1[q_], Brow2])

            def f2(i):
                p = i % 2
                q_ = i % NFB
                if i + NFB - 1 < NT:
                    fetch(i + NFB - 1)
                op(DVE, lambda: ve.scalar_tensor_tensor(out=z[i % 3][:], in0=xin[q_][:], scalar=ALPHA, in1=z[i % 3][:], op0=ALU.mult, op1=ALU.add),
                   outs=[Bz[i % 3]], ins=[Bxin[q_], Bz[i % 3]])
                mv = mvA[:, i, :]
                rs = rsA[:, i, 0:1]
                nmr = rsA[:, i, 1:2]
                for half in range(2):
                    op(DVE, lambda: ve.bn_stats(out=st[:, half, :], in_=z[i % 3][:, half * 512:(half + 1) * 512]), outs=[Bstt], ins=[Bz[i % 3]])
                op(DVE, lambda: ve.bn_aggr(out=mv, in_=st[:].rearrange("p a b -> p (a b)")), outs=[Bst_t[i]], ins=[Bstt])
                op(DVE, lambda: ve.tensor_scalar(out=rs, in0=mv[:, 1:2], scalar1=LN_EPS, scalar2=None, op0=ALU.add), outs=[Bst_t[i]], ins=[Bst_t[i]])
                op(ACT, lambda: se.activation(out=rs, in_=rs, func=AF.Sqrt), outs=[Bst_t[i]], ins=[Bst_t[i]])
                op(DVE, lambda: ve.reciprocal(out=rs, in_=rs), outs=[Bst_t[i]], ins=[Bst_t[i]])
                op(DVE, lambda: ve.tensor_scalar(out=nmr, in0=mv[:, 0:1], scalar1=rs, scalar2=-1.0, op0=ALU.mult, op1=ALU.mult),
                   outs=[Bst_t[i]], ins=[Bst_t[i]])
                op(ACT, lambda: se.activation(out=z[i % 3][:], in_=z[i % 3][:], func=AF.Identity, scale=rs, bias=nmr), outs=[Bz[i % 3]], ins=[Bz[i % 3], Bst_t[i]])

            def f3(i):
                p = i % 2
                tsl = slice(i * 128, (i + 1) * 128)
                op(DVE, lambda: ve.tensor_tensor(out=ot[p][:], in0=z[i % 3][:], in1=L2G[:], op=ALU.mult), outs=[Bot[p]], ins=[Bz[i % 3], Brow2])
                op(POOL, lambda: ge.tensor_tensor(out=ot[p][:], in0=ot[p][:], in1=L2B[:], op=ALU.add), outs=[Bot[p]], ins=[Bot[p], Brow2])
                outs_t.append(K.q_sp.dma(out_d[tsl, :], ot[p][:], ins=[Bot[p]]))

            f1(0)
            for i in range(NT):
                if i + 1 < NT:
                    f1(i + 1)
                f2(i)
                if i >= 1:
                    f3(i - 1)
            f3(NT - 1)
            for t in outs_t + list(dbg_out.values()):
                K.sp.wait(t)
            K.barrier()
            K.pop()
    return nc


_NC_CACHE = {}


def make_in_maps(inputs):
    f = lambda a: np.ascontiguousarray(np.asarray(a, dtype=np.float32))
    x = f(inputs["x"])
    c = f(inputs["c"])
    lbl = f(inputs["hgrn_lb_logits"])
    lbl_l = np.concatenate([lbl[0].reshape(4, 128).T, lbl[1].reshape(4, 128).T], axis=1)
    shared = {
        "w_ada": f(inputs["w_ada"][0]),
        "b_adaT": f(inputs["b_ada"][0].reshape(48, 128).T),
        "w_in": f(inputs["w_in"][0]),
        "bff": f(inputs["b_fox_forget"][0]),
        "lbl": f(lbl_l),
        "nw": f(inputs["hgrn_norm_w"][0]),
        "w_up_fox": f(inputs["w_up_fox"][0]),
        "w_up_hgrn": f(inputs["w_up_hgrn"][0]),
        "w_out": f(inputs["w_out"][0]),
        "ln1_g": f(inputs["ln1_g"][0]),
        "ln1_b": f(inputs["ln1_b"][0]),
        "ln2_g": f(inputs["ln2_g"][0]),
        "ln2_b": f(inputs["ln2_b"][0]),
        "w_r": f(np.concatenate([inputs["w_router_group"][0], inputs["w_router_expert"][0]], axis=1)),
        "b_r": f(np.concatenate([inputs["b_router_group"][0], inputs["b_router_expert"][0]], axis=0)),
        "w_eg": f(np.asarray(inputs["w_expert_gate"][0]).reshape(NEXP, 8, 128, 256).transpose(0, 2, 1, 3).reshape(NEXP * 128, 2048)),
        "w_eu": f(np.asarray(inputs["w_expert_up"][0]).reshape(NEXP, 8, 128, 256).transpose(0, 2, 1, 3).reshape(NEXP * 128, 2048)),
        "w_ed": f(np.asarray(inputs["w_expert_down"][0]).reshape(NEXP, 2, 128, D).transpose(0, 2, 1, 3).reshape(NEXP * 128, 2048)),
    }
    maps = []
    for b in range(8):
        m = dict(shared)
        m["x"] = f(x[b])
        m["cT"] = f(c[b].reshape(8, 128).T)
        maps.append(m)
    return maps


def kernel(**inputs):
    if "nc" not in _NC_CACHE:
        _NC_CACHE["nc"] = build()
    nc = _NC_CACHE["nc"]
    in_maps = make_in_maps(inputs)
    res = run_bass_kernel_spmd(nc, in_maps, core_ids=list(range(8)))
    out = np.stack([np.asarray(r["out"], dtype=np.float32) for r in res.results], axis=0)
    return out
```

```python
import bisect
from contextlib import ExitStack, suppress
import numpy as np
import concourse.bass as bass
import concourse.mybir as mybir
from concourse.bass_utils import run_bass_kernel_spmd

F32 = mybir.dt.float32
BF16 = mybir.dt.bfloat16
AF = mybir.ActivationFunctionType
ALU = mybir.AluOpType
AX = mybir.AxisListType

S = 2048
D = 1024
NT = 16
IN_DIM = 5640
O_FQ, O_FK, O_FV, O_FF, O_HQ, O_HF, O_HI, O_HG, O_GF, O_GH = 0, 512, 1024, 1536, 1544, 2056, 2568, 3080, 3592, 4616
ALPHA = 2.0 ** 0.25
LN_EPS = 1e-5
RMS_EPS = 1e-6
NEXP = 64
BIG = 1.0e4


class Tok:
    __slots__ = ("eng", "idx")

    def __init__(self, eng, idx):
        self.eng = eng
        self.idx = idx


class DTok:
    __slots__ = ("sem", "val", "key")

    def __init__(self, sem, val, key):
        self.sem = sem
        self.val = val
        self.key = key


class Eng:
    def __init__(self, K, name, e, self_sync=True):
        self.K = K
        self.name = name
        self.e = e
        self.sem = K.root.enter_context(K.nc.semaphore("s_" + name))
        self.n = 0
        self.cnt = 0
        self.last = None
        self.sig_idx = []
        self.sig_val = []
        self.seen = {}
        self.self_sync = self_sync

    def emit(self, ins):
        self.n += 1
        self.last = ins
        return Tok(self, self.n)

    def value_for(self, idx):
        p = bisect.bisect_left(self.sig_idx, idx)
        if p < len(self.sig_idx):
            return self.sig_val[p]
        assert self.last is not None and self.n >= idx
        self.last.then_inc(self.sem, 1)
        self.cnt += 1
        self.sig_idx.append(self.n)
        self.sig_val.append(self.cnt)
        return self.cnt

    def signal_last(self):
        if self.last is not None and (not self.sig_idx or self.sig_idx[-1] != self.n):
            self.last.then_inc(self.sem, 1)
            self.cnt += 1
            self.sig_idx.append(self.n)
            self.sig_val.append(self.cnt)

    def wait(self, tok):
        if tok is None:
            return
        if isinstance(tok, DTok):
            if self.seen.get(tok.key, 0) >= tok.val:
                return
            self.e.wait_ge(tok.sem, tok.val)
            self.seen[tok.key] = tok.val
            return
        if tok.eng is self and not self.self_sync:
            return
        src = tok.eng
        p = bisect.bisect_left(src.sig_idx, tok.idx)
        if p < len(src.sig_idx) and self.seen.get(src.name, 0) >= src.sig_val[p]:
            return
        v = src.value_for(tok.idx)
        if self.seen.get(src.name, 0) >= v:
            return
        self.e.wait_ge(src.sem, v)
        self.seen[src.name] = v


class Buf:
    def __init__(self, name=""):
        self.name = name
        self.w = None
        self.wl = []
        self.r = {}
        self.rd = []


class DmaQ:
    def __init__(self, K, name, issuer, nsem):
        self.K = K
        self.name = name
        self.I = issuer
        self.sems = [K.root.enter_context(K.nc.semaphore(f"d_{name}{i}")) for i in range(nsem)]
        self.vals = [0] * nsem
        self.rr = 0
        self.out = []

    def dma(self, out, in_, outs=(), ins=()):
        I = self.I
        for b in ins:
            I.wait(b.w)
            for t in b.wl:
                I.wait(t)
        for b in outs:
            I.wait(b.w)
            for t in b.wl:
                I.wait(t)
            for t in b.r.values():
                I.wait(t)
            for t in b.rd:
                I.wait(t)
        j = self.rr
        self.rr = (self.rr + 1) % len(self.sems)
        key = f"{self.name}{j}"
        if self.vals[j] > 0:
            I.wait(DTok(self.sems[j], self.vals[j], key))
        ins_ = I.e.dma_start(out=out, in_=in_)
        ins_.then_inc(self.sems[j], 16)
        self.vals[j] += 16
        tok = DTok(self.sems[j], self.vals[j], key)
        for b in ins:
            b.rd.append(tok)
        for b in outs:
            if isinstance(b.w, DTok):
                b.wl.append(b.w)
            b.w = tok
            b.r = {}
            b.rd = []
        return tok

    def all_toks(self):
        return [DTok(self.sems[j], self.vals[j], f"{self.name}{j}") for j in range(len(self.sems)) if self.vals[j] > 0]


class Kern:
    def __init__(self, nc, root):
        self.nc = nc
        self.root = root
        self.es = root
        self.pe = Eng(self, "pe", nc.tensor, self_sync=False)
        self.act = Eng(self, "act", nc.scalar)
        self.dve = Eng(self, "dve", nc.vector)
        self.pool = Eng(self, "pool", nc.gpsimd)
        self.sp = Eng(self, "sp", nc.sync)
        self.engs = [self.pe, self.act, self.dve, self.pool, self.sp]
        self.q_sp = DmaQ(self, "qsp", self.sp, 8)
        self.q_pl = DmaQ(self, "qpl", self.pool, 12)
        self.nbuf = 0
        nbytes = (int(nc.sbuf_bytes_remaining) - 2048) // 64 * 64
        self.arena = root.enter_context(nc.sbuf_tensor("arena", [128, nbytes // 4], F32))
        self.free = [(0, nbytes)]
        self.scopes = [[]]
        self.peak = 0
        self.nbytes = nbytes

    def _alloc(self, n):
        for idx, (o, sz) in enumerate(self.free):
            if sz >= n:
                if sz == n:
                    self.free.pop(idx)
                else:
                    self.free[idx] = (o + n, sz - n)
                used = self.nbytes - sum(z for _, z in self.free)
                self.peak = max(self.peak, used)
                return o
        raise RuntimeError(f"arena OOM need {n} free {self.free}")

    def _release(self, o, n):
        self.free.append((o, n))
        self.free.sort()
        m = []
        for o_, n_ in self.free:
            if m and m[-1][0] + m[-1][1] == o_:
                m[-1] = (m[-1][0], m[-1][1] + n_)
            else:
                m.append((o_, n_))
        self.free = m

    def sb(self, name, shape, dt, persist=False):
        parts = shape[0]
        elems = 1
        for d in shape[1:]:
            elems *= d
        esz = 2 if dt == BF16 else 4
        n = (elems * esz + 63) // 64 * 64
        o = self._alloc(n)
        v = self.arena[0:parts, o // 4:(o + n) // 4]
        if dt != F32:
            v = v.bitcast(dt)
        v = v[:, 0:elems]
        if len(shape) > 2:
            names = "abcdefg"[:len(shape) - 1]
            pat = "p (" + " ".join(names) + ") -> p " + " ".join(names)
            v = v.rearrange(pat, **{names[i]: shape[1 + i] for i in range(len(shape) - 1)})
        if persist:
            self.handles = getattr(self, "handles", {})
            self.handles[name] = (o, n)
        else:
            self.scopes[-1].append((o, n))
        return v

    def release(self, name):
        o, n = self.handles.pop(name)
        self._release(o, n)

    def push(self):
        self.scopes.append([])

    def pop(self):
        for o, n in self.scopes.pop():
            self._release(o, n)

    def op(self, eng, fn, outs=(), ins=()):
        for b in ins:
            eng.wait(b.w)
            for t in b.wl:
                eng.wait(t)
        for b in outs:
            eng.wait(b.w)
            for t in b.wl:
                eng.wait(t)
            for t in b.r.values():
                eng.wait(t)
            for t in b.rd:
                eng.wait(t)
        if eng is self.pe:
            key = tuple(id(b) for b in outs)
            if key != getattr(eng, "prev_outs", None):
                eng.signal_last()
            eng.prev_outs = key
        tok = eng.emit(fn())
        if eng is not self.pe and eng is not self.sp:
            eng.signal_last()
        for b in ins:
            b.r[eng.name] = tok
        for b in outs:
            b.w = tok
            b.wl = []
            b.r = {}
            b.rd = []
        return tok

    def barrier(self):
        toks = [Tok(e, e.n) for e in self.engs if e.n > 0]
        dt = self.q_sp.all_toks() + self.q_pl.all_toks()
        for e in self.engs:
            for t in toks:
                if t.eng is not e:
                    e.wait(t)
            for t in dt:
                e.wait(t)


class _Stop(Exception):
    pass


def build(debug=None, stop=None):
    debug = debug or []
    nc = bass.Bass("TRN2", target_bir_lowering=False)

    def din(name, shape):
        return nc.dram_tensor(name, shape, F32, kind="ExternalInput").ap()

    x_d = din("x", [S, D])
    cT_d = din("cT", [128, 8])
    wada_d = din("w_ada", [D, 6 * D])
    badaT_d = din("b_adaT", [128, 48])
    win_d = din("w_in", [D, IN_DIM])
    bff_d = din("bff", [8])
    lbl_d = din("lbl", [128, 8])
    nw_d = din("nw", [512])
    wupf_d = din("w_up_fox", [512, D])
    wuph_d = din("w_up_hgrn", [512, D])
    wout_d = din("w_out", [D, D])
    ln1g_d = din("ln1_g", [D])
    ln1b_d = din("ln1_b", [D])
    ln2g_d = din("ln2_g", [D])
    ln2b_d = din("ln2_b", [D])
    wr_d = din("w_r", [D, 72])
    br_d = din("b_r", [72])
    nexp_decl = 1 if (stop is not None and stop < 7) else NEXP
    weg_d = din("w_eg", [nexp_decl * 128, 2048])
    weu_d = din("w_eu", [nexp_decl * 128, 2048])
    wed_d = din("w_ed", [nexp_decl * 128, 2048])
    out_d = nc.dram_tensor("out", [S, D], F32, kind="ExternalOutput").ap()
    x1_d = nc.dram_tensor("x1_scratch", [S, D], F32, kind="Internal").ap()

    dbg_out = {}

    with ExitStack() as root, suppress(_Stop):
        K = Kern(nc, root)
        op = K.op
        PEe, ACT, DVE, POOL = K.pe, K.act, K.dve, K.pool
        te, se, ve, ge = nc.tensor, nc.scalar, nc.vector, nc.gpsimd

        def dump(name, ap, shape, bufs, dt=F32):
            if name not in debug:
                return
            d = nc.dram_tensor("dbg_" + name, list(shape), dt, kind="ExternalOutput").ap()
            dbg_out[name] = K.q_sp.dma(d, ap, ins=bufs)
            K.sp.wait(dbg_out[name])

        PB = [root.enter_context(nc.psum_tensor(f"pb{i}", [128, 512], F32)) for i in range(8)]
        BPB = [Buf(f"pb{i}") for i in range(8)]
        bank_rr = [0]

        def nbank(lo=0, hi=8):
            n = hi - lo
            i = lo + bank_rr[0] % n
            bank_rr[0] += 1
            return i

        Bc = Buf("const")
        ident_f = K.sb("ident_f", [128, 128], F32)
        ident_b = K.sb("ident_b", [128, 128], BF16)
        ones_f = K.sb("ones_f", [128, 128], F32)
        tri_f = K.sb("tri_f", [128, 128], F32)
        negmask_b = K.sb("negmask_b", [128, 128], BF16)
        hmask_f = K.sb("hmask_f", [128, 128], F32)
        rmask = K.sb("rmask", [128, S], F32)
        op(POOL, lambda: ge.memset(ident_f[:], 0.0), outs=[Bc])
        op(POOL, lambda: ge.affine_select(out=ident_f[:], in_=ident_f[:], compare_op=ALU.not_equal, fill=1.0,
                                          base=0, pattern=[[-1, 128]], channel_multiplier=1), outs=[Bc])
        op(POOL, lambda: ge.tensor_copy(out=ident_b[:], in_=ident_f[:]), outs=[Bc])
        op(POOL, lambda: ge.memset(ones_f[:], 1.0), outs=[Bc])
        op(POOL, lambda: ge.memset(tri_f[:], 1.0), outs=[Bc])
        op(POOL, lambda: ge.affine_select(out=tri_f[:], in_=tri_f[:], compare_op=ALU.is_ge, fill=0.0,
                                          base=0, pattern=[[1, 128]], channel_multiplier=-1), outs=[Bc])
        op(POOL, lambda: ge.memset(negmask_b[:], 0.0), outs=[Bc])
        op(POOL, lambda: ge.affine_select(out=negmask_b[:], in_=negmask_b[:], compare_op=ALU.is_ge, fill=-30000.0,
                                          base=0, pattern=[[1, 128]], channel_multiplier=-1), outs=[Bc])
        op(POOL, lambda: ge.tensor_copy(out=hmask_f[:], in_=tri_f[:]), outs=[Bc])
        op(POOL, lambda: ge.memset(hmask_f[0:64, 64:128], 0.0), outs=[Bc])
        op(POOL, lambda: ge.memset(rmask[:], 1.0), outs=[Bc])
        op(POOL, lambda: ge.memset(rmask[:, 0:S:64], 0.0), outs=[Bc])

        win_r = win_d.rearrange("(k p) n -> p k n", p=128)
        wq = K.sb("wq", [128, 8, 8, 65], BF16, persist=True)
        wk = K.sb("wk", [128, 8, 512], BF16, persist=True)
        wv = K.sb("wv", [128, 8, 512], BF16, persist=True)
        wff = K.sb("wff", [128, 8, 8], BF16, persist=True)
        Bw = Buf("wA")
        K.q_pl.dma(wv[:], win_r[:, :, O_FV:O_FV + 512], outs=[Bw])
        K.q_pl.dma(wff[:], win_r[:, :, O_FF:O_FF + 8], outs=[Bw])
        for k in range(8):
            K.q_pl.dma(wq[:, k, :, 0:64], win_r[:, k, O_FQ:O_FQ + 512].rearrange("p (h d) -> p h d", h=8), outs=[Bw])
        op(POOL, lambda: ge.memset(wq[:, :, :, 64:65], 0.0), outs=[Bw])
        K.q_pl.dma(wk[:], win_r[:, :, O_FK:O_FK + 512], outs=[Bw])

        Bada = Buf("ada")
        adaT = K.sb("adaT", [128, 48], F32)
        sc1p = K.sb("sc1p", [128, 8], F32)
        sc2p = K.sb("sc2p", [128, 8], F32)
        lb = K.sb("lb", [128, 4], F32)
        omlb = K.sb("omlb", [128, 4], F32)
        nomlb = K.sb("nomlb", [128, 4], F32)
        bffb = K.sb("bffb", [128, 8], F32)
        if True:
            K.push()
            cT_sb = K.sb("cT_sb", [128, 8], F32)
            c_act = K.sb("c_act", [128, 8], F32)
            badaT = K.sb("badaT", [128, 48], F32)
            lbl = K.sb("lbl_sb", [128, 8], F32)
            wa = [K.sb(f"wa{i}", [128, 8, 512], BF16) for i in range(3)]
            c_actb = K.sb("c_actb", [128, 8], BF16)
            Bwa = [Buf(), Buf(), Buf()]
            Bs = Buf("small")
            K.q_sp.dma(cT_sb[:], cT_d, outs=[Bs])
            K.q_sp.dma(badaT[:], badaT_d, outs=[Bs])
            K.q_sp.dma(lbl[:], lbl_d, outs=[Bs])
            K.q_sp.dma(bffb[:], bff_d.partition_broadcast(128), outs=[Bs])
            op(ACT, lambda: se.activation(out=c_act[:], in_=cT_sb[:], func=AF.Silu), outs=[Bs], ins=[Bs])
            op(ACT, lambda: se.copy(out=c_actb[:], in_=c_act[:]), outs=[Bs], ins=[Bs])
            op(DVE, lambda: ve.tensor_tensor(out=lb[:], in0=lbl[:, 0:4], in1=lbl[:, 4:8], op=ALU.subtract), outs=[Bada], ins=[Bs])
            op(ACT, lambda: se.activation(out=lb[:], in_=lb[:], func=AF.Sigmoid), outs=[Bada], ins=[Bada])
            op(DVE, lambda: ve.tensor_scalar(out=omlb[:], in0=lb[:], scalar1=-1.0, scalar2=1.0, op0=ALU.mult, op1=ALU.add), outs=[Bada], ins=[Bada])
            op(DVE, lambda: ve.tensor_scalar(out=nomlb[:], in0=lb[:], scalar1=1.0, scalar2=None, op0=ALU.subtract), outs=[Bada], ins=[Bada])
            wada_r = wada_d.rearrange("(k p) n -> p k n", p=128)
            pa = 0
            for cb in range(12):
                w_ = wa[cb % 3]
                K.q_pl.dma(w_[:], wada_r[:, :, cb * 512:(cb + 1) * 512], outs=[Bwa[cb % 3]])
                for jj in range(4):
                    j = cb * 4 + jj
                    for k in range(8):
                        op(PEe, lambda: te.matmul(PB[pa][:, j:j + 1], lhsT=w_[:, k, jj * 128:(jj + 1) * 128],
                                                  rhs=c_actb[:, k:k + 1], start=(k == 0), stop=(k == 7)),
                           outs=[BPB[pa]], ins=[Bwa[cb % 3], Bs])
            op(DVE, lambda: ve.tensor_tensor(out=adaT[:], in0=PB[pa][:, 0:48], in1=badaT[:], op=ALU.add), outs=[Bada], ins=[BPB[pa], Bs])
            op(DVE, lambda: ve.tensor_scalar(out=sc1p[:], in0=adaT[:, 8:16], scalar1=1.0, scalar2=None, op0=ALU.add), outs=[Bada], ins=[Bada])
            op(DVE, lambda: ve.tensor_scalar(out=sc2p[:], in0=adaT[:, 32:40], scalar1=1.0, scalar2=None, op0=ALU.add), outs=[Bada], ins=[Bada])
            dump("adaT", adaT[:], [128, 48], [Bada])
            K.barrier()
            K.pop()
            if stop == 0:
                raise _Stop()

        def bcast_rows(dst, Bdst, colsrc):
            for half in range(2):
                dg = K.sb("dg", [128, 512], F32)
                Bdg = Buf()
                for jj in range(4):
                    j = half * 4 + jj
                    op(DVE, lambda: ve.tensor_scalar(out=dg[:, jj * 128:(jj + 1) * 128], in0=ident_f[:], scalar1=colsrc[:, j:j + 1], scalar2=None,
                                                     op0=ALU.mult), outs=[Bdg], ins=[Bc, Bada])
                bk = nbank(0, 8)
                op(PEe, lambda: te.matmul(PB[bk][:, :], lhsT=ones_f[:], rhs=dg[:], start=True, stop=True), outs=[BPB[bk]], ins=[Bdg, Bc])
                op(ACT, lambda: se.copy(out=dst[:, half * 512:(half + 1) * 512], in_=PB[bk][:, :]), outs=[Bdst], ins=[BPB[bk]])

        hT = K.sb("hT", [128, 8, S], BF16, persist=True)
        BhT = [Buf(f"hT{i}") for i in range(NT)]
        if True:
            K.push()
            SC1row = K.sb("SC1row", [128, D], F32)
            SH1row = K.sb("SH1row", [128, D], F32)
            Brow1 = Buf("rows_s1")
            K.push()
            bcast_rows(SC1row, Brow1, sc1p)
            bcast_rows(SH1row, Brow1, adaT[:, 0:8])
            K.barrier()
            K.pop()
            xin = [K.sb(f"xin{i}", [128, D], F32) for i in range(3)]
            Bxin = [Buf(), Buf(), Buf()]
            hm = [K.sb(f"hm{i}", [128, D], F32) for i in range(2)]
            Bhm = [Buf(), Buf()]
            hb = [K.sb(f"hb{i}", [128, D], BF16) for i in range(2)]
            Bhb = [Buf(), Buf()]
            for i in range(2):
                K.q_sp.dma(xin[i][:], x_d[i * 128:(i + 1) * 128, :], outs=[Bxin[i]])
            for i in range(NT):
                p = i % 2
                xb = xin[i % 3]
                if i + 2 < NT:
                    K.q_sp.dma(xin[(i + 2) % 3][:], x_d[(i + 2) * 128:(i + 3) * 128, :], outs=[Bxin[(i + 2) % 3]])
                op(DVE, lambda: ve.tensor_tensor(out=hm[p][:], in0=xb[:], in1=SC1row[:], op=ALU.mult), outs=[Bhm[p]], ins=[Bxin[i % 3], Brow1])
                op(DVE, lambda: ve.tensor_tensor(out=hb[p][:], in0=hm[p][:], in1=SH1row[:], op=ALU.add), outs=[Bhb[p]], ins=[Bhm[p], Brow1])
                pbf = PB[p][:, :].bitcast(BF16)
                for c in range(8):
                    op(PEe, lambda: te.transpose(pbf[:, c * 128:(c + 1) * 128], hb[p][:, c * 128:(c + 1) * 128], ident_b[:]),
                       outs=[BPB[p]], ins=[Bhb[p], Bc])
                op(ACT, lambda: se.copy(out=hT[:, :, i * 128:(i + 1) * 128], in_=pbf[:, :].rearrange("p (c t) -> p c t", c=8)),
                   outs=[BhT[i]], ins=[BPB[p]])
            dump("hT", hT[:], [128, 8, S], BhT, BF16)
            K.barrier()
            K.pop()
            if stop == 1:
                raise _Stop()


        def hT_blk(tb):
            return BhT[4 * tb:4 * tb + 4]

        yfoxT = K.sb("yfoxT", [128, 4, S], BF16, persist=True)
        ByfT = Buf("yfoxT")
        if True:
            K.push()
            v_aug = K.sb("v_aug", [128, NT, 8, 65], BF16)
            Bv = Buf("v_aug")
            op(POOL, lambda: ge.memset(v_aug[:, :, :, 64:65], 1.0), outs=[Bv])
            FFB = 7
            for i in range(NT):
                bk = nbank(0, 4)
                for k in range(8):
                    op(PEe, lambda: te.matmul(PB[bk][:, :], lhsT=hT[:, k, i * 128:(i + 1) * 128], rhs=wv[:, k, :], start=(k == 0), stop=(k == 7)),
                       outs=[BPB[bk]], ins=[BhT[i], Bw])
                src = PB[bk][:, :].rearrange("p (h d) -> p h d", h=8)
                if i % 2 == 0:
                    op(ACT, lambda: se.copy(out=v_aug[:, i, :, 0:64], in_=src), outs=[Bv], ins=[BPB[bk]])
                else:
                    op(DVE, lambda: ve.tensor_copy(out=v_aug[:, i, :, 0:64], in_=src), outs=[Bv], ins=[BPB[bk]])
                for k in range(8):
                    op(PEe, lambda: te.matmul(PB[FFB][:, i * 8:(i + 1) * 8], lhsT=hT[:, k, i * 128:(i + 1) * 128], rhs=wff[:, k, :], start=(k == 0), stop=(k == 7)),
                       outs=[BPB[FFB]], ins=[BhT[i], Bw])
            if stop == 1.1:
                K.barrier()
                raise _Stop()
            lfn = K.sb("lfn", [128, NT, 8], F32)
            Blf = Buf("lf")
            negcum = K.sb("negcum", [128, NT, 8], F32)
            carry = K.sb("carry", [128, NT, 8], F32)
            tot = K.sb("tot", [128, NT, 8], F32)
            Bnc = Buf("negcum")
            op(DVE, lambda: ve.tensor_tensor(out=lfn[:], in0=PB[FFB][:, 0:128].rearrange("p (i h) -> p i h", h=8),
                                             in1=bffb[:].unsqueeze(1).to_broadcast([128, NT, 8]), op=ALU.add), outs=[Blf], ins=[BPB[FFB], Bada])
            op(ACT, lambda: se.activation(out=lfn[:], in_=lfn[:], func=AF.Exp, scale=-1.0), outs=[Blf], ins=[Blf])
            op(ACT, lambda: se.activation(out=lfn[:], in_=lfn[:], func=AF.Ln, bias=1.0, scale=1.0), outs=[Blf], ins=[Blf])
            lfn2 = lfn[:].rearrange("p i h -> p (i h)")
            op(PEe, lambda: te.matmul(PB[4][:, 0:128], lhsT=tri_f[:], rhs=lfn2, start=True, stop=True), outs=[BPB[4]], ins=[Blf, Bc])
            op(PEe, lambda: te.matmul(PB[5][:, 0:128], lhsT=ones_f[:], rhs=lfn2, start=True, stop=True), outs=[BPB[5]], ins=[Blf, Bc])
            Bcar = Buf("carry")
            op(DVE, lambda: ve.tensor_copy(out=tot[:].rearrange("p i h -> p (i h)"), in_=PB[5][:, 0:128]), outs=[Bcar], ins=[BPB[5]])
            op(DVE, lambda: ve.memset(carry[:, 0, :], 0.0), outs=[Bcar], ins=[Bcar])
            for i in range(1, NT):
                op(DVE, lambda: ve.tensor_tensor(out=carry[:, i, :], in0=carry[:, i - 1, :], in1=tot[:, i - 1, :], op=ALU.add), outs=[Bcar], ins=[Bcar])
            op(DVE, lambda: ve.tensor_tensor(out=negcum[:].rearrange("p i h -> p (i h)"), in0=PB[4][:, 0:128],
                                             in1=carry[:].rearrange("p i h -> p (i h)"), op=ALU.add), outs=[Bnc], ins=[BPB[4], Bcar])
            dump("negcum", negcum[:], [128, NT, 8], [Bnc])
            if stop == 1.2:
                K.barrier()
                raise _Stop()
            Zr = K.sb("Zr", [128, NT, 8, 65], BF16)
            BZr = Buf("Zr")
            op(POOL, lambda: ge.memset(Zr[:], 0.0), outs=[BZr])
            op(DVE, lambda: ve.tensor_scalar(out=Zr[:, :, :, 64:65], in0=negcum[:].unsqueeze(3), scalar1=-1.0, scalar2=None, op0=ALU.mult),
               outs=[BZr], ins=[Bnc, BZr])
            qscale = K.sb("qscale", [65, 1], F32)
            Bqs = Buf("qscale")
            op(POOL, lambda: ge.memset(qscale[:], 1.0), outs=[Bqs])
            op(POOL, lambda: ge.memset(qscale[0:64, :], 0.125), outs=[Bqs])
            if stop == 1.3:
                K.barrier()
                raise _Stop()
            qa = [K.sb(f"qa{i}", [65, S], BF16) for i in range(2)]
            ka = [K.sb(f"ka{i}", [65, S], BF16) for i in range(2)]
            Bqa = [Buf(), Buf()]
            Bka = [Buf(), Buf()]
            for i in range(2):
                op(POOL, lambda: ge.memset(ka[i][:], 1.0), outs=[Bka[i]])
            yfox = K.sb("yfox", [128, NT, 512], BF16)
            Byf = [Buf(f"yf{i}") for i in range(NT)]
            pts = [K.sb(f"pt{i}", [128, 512], BF16) for i in range(3)]
            Bpt = [Buf() for _ in range(3)]
            rec = K.sb("rec", [128, 8], F32)
            Brec = Buf("rec")
            ptn = [0]
            SB_ = [0, 1, 2, 3]
            ACCB = [4, 5]
            Bacc = [[Buf() for _ in range(4)] for _ in range(2)]
            def proj_groups(h):
                q_, k_ = qa[h % 2], ka[h % 2]
                Bq, Bk = Bqa[h % 2], Bka[h % 2]
                gs = []
                for tb in range(4):
                    def gq(tb=tb):
                        bk = 6 + (tb % 2)
                        for k in range(8):
                            op(PEe, lambda: te.matmul(PB[bk][0:65, :], lhsT=wq[:, k, h, :], rhs=hT[:, k, tb * 512:(tb + 1) * 512],
                                                      start=(k == 0), stop=False), outs=[BPB[bk]], ins=hT_blk(tb) + [Bw])
                        for ii in range(4):
                            op(PEe, lambda: te.matmul(PB[bk][0:65, ii * 128:(ii + 1) * 128], lhsT=Zr[:, tb * 4 + ii, h, :], rhs=ident_b[:],
                                                      start=False, stop=(ii == 3)), outs=[BPB[bk]], ins=[BZr, Bc])
                        op(DVE, lambda: ve.tensor_scalar(out=q_[0:65, tb * 512:(tb + 1) * 512], in0=PB[bk][0:65, :], scalar1=qscale[:, 0:1],
                                                         scalar2=None, op0=ALU.mult), outs=[Bq], ins=[BPB[bk], Bqs])

                    def gk(tb=tb):
                        bk2 = 6 + ((tb + 1) % 2)
                        for k in range(8):
                            op(PEe, lambda: te.matmul(PB[bk2][0:64, :], lhsT=wk[:, k, h * 64:(h + 1) * 64], rhs=hT[:, k, tb * 512:(tb + 1) * 512],
                                                      start=(k == 0), stop=(k == 7)), outs=[BPB[bk2]], ins=hT_blk(tb) + [Bw])
                        op(DVE, lambda: ve.tensor_copy(out=k_[0:64, tb * 512:(tb + 1) * 512], in_=PB[bk2][0:64, :]), outs=[Bk], ins=[BPB[bk2]])
                    gs += [gq, gk]
                return gs

            for g_ in proj_groups(0):
                g_()
            for h in range(8):
                q_, k_ = qa[h % 2], ka[h % 2]
                Bq, Bk = Bqa[h % 2], Bka[h % 2]
                pending = proj_groups(h + 1) if h + 1 < 8 else []
                itc = [0]

                if stop == 1.4:
                    K.barrier()
                    raise _Stop()

                def qk(I, j):
                    jj = j - 4 * I
                    c0 = max(jj, 0) * 128
                    N = 512 - c0
                    bk = SB_[nbank(0, 4)]
                    ksl = k_[0:65, j * 128:(j + 1) * 128]
                    if jj < 0:
                        op(PEe, lambda: te.matmul(PB[bk][:, 0:N], lhsT=ksl, rhs=q_[0:65, I * 512 + c0:(I + 1) * 512], start=True, stop=True),
                           outs=[BPB[bk]], ins=[Bq, Bk])
                    else:
                        op(PEe, lambda: te.matmul(PB[bk][:, 0:128], lhsT=ksl, rhs=q_[0:65, I * 512 + c0:I * 512 + c0 + 128], start=True, stop=False),
                           outs=[BPB[bk]], ins=[Bq, Bk])
                        op(PEe, lambda: te.matmul(PB[bk][:, 0:128], lhsT=ident_b[:], rhs=negmask_b[:], start=False, stop=True),
                           outs=[BPB[bk]], ins=[Bc])
                        if N > 128:
                            op(PEe, lambda: te.matmul(PB[bk][:, 128:N], lhsT=ksl, rhs=q_[0:65, I * 512 + c0 + 128:(I + 1) * 512], start=True, stop=True),
                               outs=[BPB[bk]], ins=[Bq, Bk])
                    return bk, c0, N, jj

                for I in range(4):
                    ab = ACCB[I % 2]
                    Ba = Bacc[I % 2]
                    nj = 4 * I + 4
                    pend = qk(I, 0)
                    for j in range(nj):
                        bk, c0, N, jj = pend
                        if j + 1 < nj:
                            pend = qk(I, j + 1)
                        pi = ptn[0] % 3
                        ptn[0] += 1
                        pt = pts[pi]
                        op(ACT, lambda: se.activation(out=pt[:, 0:N], in_=PB[bk][:, 0:N], func=AF.Exp, bias=negcum[:, j, h:h + 1], scale=1.0),
                           outs=[Bpt[pi]], ins=[BPB[bk], Bnc])
                        for ii in range(max(jj, 0), 4):
                            i = 4 * I + ii
                            op(PEe, lambda: te.matmul(PB[ab][:, ii * 65:(ii + 1) * 65], lhsT=pt[:, ii * 128 - c0:ii * 128 - c0 + 128],
                                                      rhs=v_aug[:, j, h, :], start=(j == 0 and ii == 0), stop=(j == i), skip_group_check=True),
                               outs=[BPB[ab]], ins=[Bpt[pi], Bv])
                        itc[0] += 1
                        if itc[0] % 5 == 0 and pending:
                            pending.pop(0)()
                    for ii in range(4):
                        i = 4 * I + ii
                        op(DVE, lambda: ve.reciprocal(out=rec[:, ii:ii + 1], in_=PB[ab][:, ii * 65 + 64:ii * 65 + 65]), outs=[Brec], ins=[BPB[ab]])
                        op(DVE, lambda: ve.tensor_scalar(out=yfox[:, i, h * 64:(h + 1) * 64], in0=PB[ab][:, ii * 65:ii * 65 + 64],
                                                         scalar1=rec[:, ii:ii + 1], scalar2=None, op0=ALU.mult), outs=[Byf[i]], ins=[BPB[ab], Brec])
                    if I == 3:
                        while pending:
                            pending.pop(0)()
            if stop == 1.5:
                K.barrier()
                raise _Stop()
            dump("yfox", yfox[:], [128, NT, 512], Byf, BF16)
            for i in range(NT):
                bk = nbank(0, 4)
                pbf = PB[bk][:, :].bitcast(BF16)
                for c4 in range(4):
                    op(PEe, lambda: te.transpose(pbf[:, c4 * 128:(c4 + 1) * 128], yfox[:, i, c4 * 128:(c4 + 1) * 128], ident_b[:]),
                       outs=[BPB[bk]], ins=[Byf[i], Bc])
                src = pbf[:, 0:512].rearrange("p (c t) -> p c t", c=4)
                if i % 2 == 0:
                    op(ACT, lambda: se.copy(out=yfoxT[:, :, i * 128:(i + 1) * 128], in_=src), outs=[ByfT], ins=[BPB[bk]])
                else:
                    op(DVE, lambda: ve.tensor_copy(out=yfoxT[:, :, i * 128:(i + 1) * 128], in_=src), outs=[ByfT], ins=[BPB[bk]])
            K.barrier()
            K.pop()
            for nm_ in ("wq", "wk", "wv", "wff"):
                K.release(nm_)
            if stop == 2:
                raise _Stop()

        oT = K.sb("oT", [128, 4, S], BF16, persist=True)
        BoT = Buf("oT")
        if True:
            K.push()
            nwrow = K.sb("nwrow", [128, 512], F32)
            Bnw = Buf("nw")
            K.q_sp.dma(nwrow[:], nw_d.partition_broadcast(128), outs=[Bnw])
            whq = K.sb("whq", [128, 8, 128], BF16)
            whf = K.sb("whf", [128, 8, 128], BF16)
            whv = K.sb("whv", [128, 8, 256], BF16)
            Bwh = Buf("wh")
            T = [K.sb(f"T{i}", [128, S], F32) for i in range(5)]
            BT = [Buf(f"T{i}") for i in range(5)]
            qeT = K.sb("qeT", [128, S], BF16)
            keT = K.sb("keT", [128, S], BF16)
            klT = K.sb("klT", [128, S], BF16)
            Bqe, Bke, BklT = Buf("qe"), Buf("ke"), Buf("klT")
            kl = K.sb("kl", [128, NT, 128], BF16)
            Bkl = Buf("kl")
            vh = K.sb("vh", [128, NT, 128], BF16)
            Bvh = Buf("vh")
            nwsg = K.sb("nwsg", [128, NT, 128], F32)
            Bns = Buf("nwsg")
            sgt = [K.sb(f"sgt{i}", [128, 128], F32) for i in range(2)]
            Bsgt = [Buf(), Buf()]
            Dd = K.sb("Dd", [128, 32], F32)
            BDd = Buf("Dd")
            sf = [K.sb(f"state_f{i}", [128, 128], F32) for i in range(2)]
            Bsf = [Buf("sf0"), Buf("sf1")]
            U_sb = K.sb("U_sb", [128, 32, 128], F32)
            BU = [Buf(f"U{i}") for i in range(8)]
            stbf = K.sb("stbf", [128, 33, 128], BF16)
            Bsb = [Buf(f"stbf{c}") for c in range(33)]
            scm = [K.sb(f"scm{i}", [128, 128], BF16) for i in range(4)]
            Bscm = [Buf() for _ in range(4)]
            ofin = K.sb("ofin", [128, NT, 128], BF16)
            Bof = [Buf(f"of{i}") for i in range(NT)]
            ssq = K.sb("ssq", [128, NT, 2], F32)
            rstd = K.sb("rstd", [128, NT, 2], F32)
            junk = K.sb("junk", [128, 64], F32)
            Bss = [Buf(f"ssq{i}") for i in range(NT)]
            Bjk = Buf("junk")
            scn = [0]
            for pr in range(4):
                cs = pr * 128
                K.q_pl.dma(whq[:], win_r[:, :, O_HQ + cs:O_HQ + cs + 128], outs=[Bwh])
                K.q_pl.dma(whf[:], win_r[:, :, O_HF + cs:O_HF + cs + 128], outs=[Bwh])
                K.q_pl.dma(whv[:, :, 0:128], win_r[:, :, O_HI + cs:O_HI + cs + 128], outs=[Bwh])
                K.q_pl.dma(whv[:, :, 128:256], win_r[:, :, O_HG + cs:O_HG + cs + 128], outs=[Bwh])
                for tb in range(4):
                    sl = slice(tb * 512, (tb + 1) * 512)
                    bq = nbank(0, 4)
                    for k in range(8):
                        op(PEe, lambda: te.matmul(PB[bq][:, :], lhsT=whq[:, k, :], rhs=hT[:, k, sl], start=(k == 0), stop=(k == 7)),
                           outs=[BPB[bq]], ins=hT_blk(tb) + [Bwh])
                    bf_ = nbank(0, 4)
                    for k in range(8):
                        op(PEe, lambda: te.matmul(PB[bf_][:, :], lhsT=whf[:, k, :], rhs=hT[:, k, sl], start=(k == 0), stop=(k == 7)),
                           outs=[BPB[bf_]], ins=hT_blk(tb) + [Bwh])
                    op(ACT, lambda: se.activation(out=T[0][:, sl], in_=PB[bq][:, :], func=AF.Silu), outs=[BT[0]], ins=[BPB[bq]])
                    op(ACT, lambda: se.activation(out=T[1][:, sl], in_=PB[bf_][:, :], func=AF.Sigmoid), outs=[BT[1]], ins=[BPB[bf_]])
                op(ACT, lambda: se.activation(out=T[2][:], in_=T[1][:], func=AF.Ln, scale=omlb[:, pr:pr + 1], bias=lb[:, pr:pr + 1]),
                   outs=[BT[2]], ins=[BT[1], Bada])
                op(DVE, lambda: ve.tensor_scalar(out=T[3][:], in0=T[1][:], scalar1=nomlb[:, pr:pr + 1], scalar2=omlb[:, pr:pr + 1],
                                                 op0=ALU.mult, op1=ALU.add), outs=[BT[3]], ins=[BT[1], Bada])
                op(DVE, lambda: ve.tensor_tensor_scan(out=T[4][:], data0=rmask[:], data1=T[2][:], initial=0.0, op0=ALU.mult, op1=ALU.add),
                   outs=[BT[4]], ins=[BT[2], Bc])
                if pr == 0:
                    dump("bcum", T[4][:], [128, S], [BT[4]])
                op(ACT, lambda: se.activation(out=T[1][:], in_=T[4][:], func=AF.Exp), outs=[BT[1]], ins=[BT[4]])
                op(ACT, lambda: se.activation(out=T[2][:], in_=T[4][:], func=AF.Exp, scale=-1.0), outs=[BT[2]], ins=[BT[4]])
                op(DVE, lambda: ve.tensor_tensor(out=qeT[:], in0=T[0][:], in1=T[1][:], op=ALU.mult), outs=[Bqe], ins=[BT[0], BT[1]])
                op(POOL, lambda: ge.tensor_tensor(out=keT[:], in0=T[3][:], in1=T[2][:], op=ALU.mult), outs=[Bke], ins=[BT[3], BT[2]])
                op(DVE, lambda: ve.tensor_copy(out=Dd[:], in_=T[1][:, 63:S:64]), outs=[BDd], ins=[BT[1]])
                op(DVE, lambda: ve.tensor_tensor(out=T[0][:].rearrange("p (c t) -> p c t", t=64),
                                                 in0=T[4][:, 63:S:64].unsqueeze(2).to_broadcast([128, 32, 64]),
                                                 in1=T[4][:].rearrange("p (c t) -> p c t", t=64), op=ALU.subtract), outs=[BT[0]], ins=[BT[4]])
                op(ACT, lambda: se.activation(out=T[1][:], in_=T[0][:], func=AF.Exp), outs=[BT[1]], ins=[BT[0]])
                op(POOL, lambda: ge.tensor_tensor(out=klT[:], in0=T[3][:], in1=T[1][:], op=ALU.mult), outs=[BklT], ins=[BT[3], BT[1]])
                for g in range(4):
                    bk = nbank(0, 4)
                    pbf = PB[bk][:, :].bitcast(BF16)
                    for ii in range(4):
                        i = g * 4 + ii
                        op(PEe, lambda: te.transpose(pbf[:, ii * 128:(ii + 1) * 128], klT[:, i * 128:(i + 1) * 128], ident_b[:]),
                           outs=[BPB[bk]], ins=[BklT, Bc])
                    op(DVE, lambda: ve.tensor_copy(out=kl[:, g * 4:(g + 1) * 4, :], in_=pbf[:, 0:512].rearrange("p (i c) -> p i c", c=128)),
                       outs=[Bkl], ins=[BPB[bk]])
                for i in range(NT):
                    bk = nbank(0, 4)
                    for k in range(8):
                        op(PEe, lambda: te.matmul(PB[bk][:, 0:256], lhsT=hT[:, k, i * 128:(i + 1) * 128], rhs=whv[:, k, :], start=(k == 0), stop=(k == 7)),
                           outs=[BPB[bk]], ins=[BhT[i], Bwh])
                    op(ACT, lambda: se.copy(out=vh[:, i, :], in_=PB[bk][:, 0:128]), outs=[Bvh], ins=[BPB[bk]])
                    op(ACT, lambda: se.activation(out=sgt[i % 2][:], in_=PB[bk][:, 128:256], func=AF.Silu), outs=[Bsgt[i % 2]], ins=[BPB[bk]])
                    op(POOL, lambda: ge.tensor_tensor(out=nwsg[:, i, :], in0=sgt[i % 2][:], in1=nwrow[:, cs:cs + 128], op=ALU.mult),
                       outs=[Bns], ins=[Bsgt[i % 2], Bnw])
                U4 = U_sb[:].rearrange("p (t h) v -> p t h v", h=2)
                for g4 in range(4):
                    for half in range(2):
                        ub = 4 + 2 * (g4 % 2) + half
                        rows = slice(half * 64, half * 64 + 64)
                        for tt in range(4):
                            i = g4 * 4 + tt
                            op(PEe, lambda: te.matmul(PB[ub][:, tt * 128:(tt + 1) * 128], lhsT=kl[rows, i, :], rhs=vh[rows, i, :], start=True, stop=True),
                               outs=[BPB[ub]], ins=[Bkl, Bvh])
                    for half in range(2):
                        ub = 4 + 2 * (g4 % 2) + half
                        op(ACT, lambda: se.copy(out=U4[:, g4 * 4:(g4 + 1) * 4, half, :], in_=PB[ub][:, :].rearrange("p (c v) -> p c v", c=4)),
                           outs=[BU[g4 * 2], BU[g4 * 2 + 1]], ins=[BPB[ub]])
                op(POOL, lambda: ge.memset(sf[0][:], 0.0), outs=[Bsf[0]])
                op(POOL, lambda: ge.memset(sf[1][:], 0.0), outs=[Bsf[1]])
                op(POOL, lambda: ge.memset(stbf[:, 0, :], 0.0), outs=[Bsb[0]])
                for c in range(32):
                    src_, dst_ = sf[c % 2], sf[(c + 1) % 2]
                    for hh in range(2):
                        r = slice(hh * 64, hh * 64 + 64)
                        op(DVE, lambda: ve.scalar_tensor_tensor(out=dst_[r, r], in0=src_[r, r], scalar=Dd[r, c:c + 1],
                                                                in1=U_sb[r, c, hh * 64:hh * 64 + 64], op0=ALU.mult, op1=ALU.add),
                           outs=[Bsf[(c + 1) % 2]], ins=[Bsf[c % 2], BDd, BU[c // 4]])
                    op(ACT, lambda: se.copy(out=stbf[:, c + 1, :], in_=dst_[:, :]), outs=[Bsb[c + 1]], ins=[Bsf[(c + 1) % 2]])

                def o1(i):
                    tsl = slice(i * 128, (i + 1) * 128)
                    for hh in range(2):
                        r = slice(hh * 64, hh * 64 + 64)
                        bk = nbank(0, 4)
                        op(PEe, lambda: te.matmul(PB[bk][:, 0:128], lhsT=keT[r, tsl], rhs=qeT[r, tsl], start=True, stop=True),
                           outs=[BPB[bk]], ins=[Bke, Bqe])
                        si = (2 * i + hh) % 4
                        op(DVE, lambda: ve.tensor_tensor(out=scm[si][:], in0=PB[bk][:, 0:128], in1=hmask_f[:], op=ALU.mult),
                           outs=[Bscm[si]], ins=[BPB[bk], Bc])

                def o2(i):
                    ob = 6 + (i % 2)
                    for half in range(2):
                        c = 2 * i + half
                        rows = slice(half * 64, half * 64 + 64)
                        op(PEe, lambda: te.matmul(PB[ob][rows, 0:128], lhsT=qeT[:, c * 64:(c + 1) * 64], rhs=stbf[:, c, :], start=True, stop=False),
                           outs=[BPB[ob]], ins=[Bqe, Bsb[c]])
                    for hh in range(2):
                        si = (2 * i + hh) % 4
                        op(PEe, lambda: te.matmul(PB[ob][:, hh * 64:(hh + 1) * 64], lhsT=scm[si][:], rhs=vh[:, i, hh * 64:(hh + 1) * 64],
                                                  start=False, stop=(hh == 1)), outs=[BPB[ob]], ins=[Bscm[si], Bvh])
                    for hh in range(2):
                        op(ACT, lambda: se.activation(out=junk[:], in_=PB[ob][:, hh * 64:(hh + 1) * 64], func=AF.Square, accum_out=ssq[:, i, hh:hh + 1]),
                           outs=[Bss[i], Bjk], ins=[BPB[ob]])
                    op(DVE, lambda: ve.tensor_scalar(out=rstd[:, i, :], in0=ssq[:, i, :], scalar1=1.0 / 64.0, scalar2=RMS_EPS, op0=ALU.mult, op1=ALU.add),
                       outs=[Bss[i]], ins=[Bss[i]])
                    op(ACT, lambda: se.activation(out=rstd[:, i, :], in_=rstd[:, i, :], func=AF.Sqrt), outs=[Bss[i]], ins=[Bss[i]])
                    op(DVE, lambda: ve.reciprocal(out=rstd[:, i, :], in_=rstd[:, i, :]), outs=[Bss[i]], ins=[Bss[i]])
                    for hh in range(2):
                        op(DVE, lambda: ve.scalar_tensor_tensor(out=ofin[:, i, hh * 64:(hh + 1) * 64], in0=PB[ob][:, hh * 64:(hh + 1) * 64],
                                                                scalar=rstd[:, i, hh:hh + 1], in1=nwsg[:, i, hh * 64:(hh + 1) * 64],
                                                                op0=ALU.mult, op1=ALU.mult), outs=[Bof[i]], ins=[BPB[ob], Bss[i], Bns])

                o1(0)
                for i in range(NT):
                    if i + 1 < NT:
                        o1(i + 1)
                    o2(i)
                if pr == 0:
                    dump("ofin", ofin[:], [128, NT, 128], Bof, BF16)
                for g in range(4):
                    bk = nbank(0, 4)
                    pbf = PB[bk][:, :].bitcast(BF16)
                    for ii in range(4):
                        i = g * 4 + ii
                        op(PEe, lambda: te.transpose(pbf[:, ii * 128:(ii + 1) * 128], ofin[:, i, :], ident_b[:]), outs=[BPB[bk]], ins=[Bof[i], Bc])
                    op(ACT, lambda: se.copy(out=oT[:, pr, g * 512:(g + 1) * 512], in_=pbf[:, 0:512]), outs=[BoT], ins=[BPB[bk]])
            K.barrier()
            K.pop()
            if stop == 3:
                raise _Stop()

        mergedT = K.sb("mergedT", [128, 8, S], BF16, persist=True)
        BmT = [Buf(f"mT{i}") for i in range(4)]
        if True:
            K.push()
            wupf = K.sb("wupf", [128, 4, D], BF16)
            wuph = K.sb("wuph", [128, 4, D], BF16)
            wgf = K.sb("wgf", [128, 8, D], BF16)
            wgh = K.sb("wgh", [128, 8, D], BF16)
            Bwc = Buf("wC")
            K.q_pl.dma(wupf[:], wupf_d.rearrange("(k p) n -> p k n", p=128), outs=[Bwc])
            K.q_pl.dma(wuph[:], wuph_d.rearrange("(k p) n -> p k n", p=128), outs=[Bwc])
            for g in range(2):
                K.q_pl.dma(wgf[:, :, g * 512:(g + 1) * 512], win_r[:, :, O_GF + g * 512:O_GF + (g + 1) * 512], outs=[Bwc])
                K.q_pl.dma(wgh[:, :, g * 512:(g + 1) * 512], win_r[:, :, O_GH + g * 512:O_GH + (g + 1) * 512], outs=[Bwc])
            tmp = [[K.sb(f"mt{a}{i}", [128, 512], F32) for i in range(2)] for a in range(4)]
            Btmp = [[Buf() for i in range(2)] for a in range(4)]
            n = 0
            for dc in range(8):
                dsl = slice(dc * 128, (dc + 1) * 128)
                for tb in range(4):
                    sl = slice(tb * 512, (tb + 1) * 512)
                    p = n % 2
                    n += 1
                    b_uf, b_uh, b_gf, b_gh = 4 * p, 4 * p + 1, 4 * p + 2, 4 * p + 3
                    for k in range(8):
                        op(PEe, lambda: te.matmul(PB[b_gf][:, :], lhsT=wgf[:, k, dsl], rhs=hT[:, k, sl], start=(k == 0), stop=(k == 7)),
                           outs=[BPB[b_gf]], ins=hT_blk(tb) + [Bwc])
                    for k in range(8):
                        op(PEe, lambda: te.matmul(PB[b_gh][:, :], lhsT=wgh[:, k, dsl], rhs=hT[:, k, sl], start=(k == 0), stop=(k == 7)),
                           outs=[BPB[b_gh]], ins=hT_blk(tb) + [Bwc])
                    for k in range(4):
                        op(PEe, lambda: te.matmul(PB[b_uf][:, :], lhsT=wupf[:, k, dsl], rhs=yfoxT[:, k, sl], start=(k == 0), stop=(k == 3)),
                           outs=[BPB[b_uf]], ins=[ByfT, Bwc])
                    for k in range(4):
                        op(PEe, lambda: te.matmul(PB[b_uh][:, :], lhsT=wuph[:, k, dsl], rhs=oT[:, k, sl], start=(k == 0), stop=(k == 3)),
                           outs=[BPB[b_uh]], ins=[BoT, Bwc])
                    op(ACT, lambda: se.activation(out=tmp[0][p][:], in_=PB[b_gf][:, :], func=AF.Sigmoid), outs=[Btmp[0][p]], ins=[BPB[b_gf]])
                    op(ACT, lambda: se.activation(out=tmp[1][p][:], in_=PB[b_gh][:, :], func=AF.Sigmoid), outs=[Btmp[1][p]], ins=[BPB[b_gh]])
                    op(DVE, lambda: ve.tensor_tensor(out=tmp[2][p][:], in0=PB[b_uf][:, :], in1=tmp[0][p][:], op=ALU.mult),
                       outs=[Btmp[2][p]], ins=[BPB[b_uf], Btmp[0][p]])
                    op(DVE, lambda: ve.tensor_tensor(out=tmp[3][p][:], in0=PB[b_uh][:, :], in1=tmp[1][p][:], op=ALU.mult),
                       outs=[Btmp[3][p]], ins=[BPB[b_uh], Btmp[1][p]])
                    op(POOL, lambda: ge.tensor_tensor(out=mergedT[:, dc, sl], in0=tmp[2][p][:], in1=tmp[3][p][:], op=ALU.add),
                       outs=[BmT[tb]], ins=[Btmp[2][p], Btmp[3][p]])
            dump("mergedT", mergedT[:], [128, 8, S], BmT, BF16)
            K.barrier()
            K.pop()
            if stop == 4:
                raise _Stop()
            K.release("hT")
            K.release("yfoxT")
            K.release("oT")

        G2row = K.sb("G2row", [128, D], F32)
        L2G = K.sb("L2G", [128, D], F32)
        L2B = K.sb("L2B", [128, D], F32)
        Brow2 = Buf("rows2")
        K.q_sp.dma(L2G[:], ln2g_d.partition_broadcast(128), outs=[Brow2])
        K.q_sp.dma(L2B[:], ln2b_d.partition_broadcast(128), outs=[Brow2])

        h2T = K.sb("h2T", [128, 8, S], BF16, persist=True)
        h2tok = K.sb("h2tok", [128, NT, D], BF16, persist=True)
        Bh2k = [Buf(f"h2k{i}") for i in range(NT)]
        Bh2T = [Buf(f"h2T{i}") for i in range(NT)]
        Bx1d = [Buf(f"x1d{i}") for i in range(NT)]
        if True:
            K.push()
            wout = K.sb("wout", [128, 8, D], BF16)
            Bwo = Buf("wout")
            K.q_pl.dma(wout[:], wout_d.rearrange("(k p) n -> p k n", p=128), outs=[Bwo])
            G1row = K.sb("G1row", [128, D], F32)
            L1G = K.sb("L1G", [128, D], F32)
            L1B = K.sb("L1B", [128, D], F32)
            A2 = K.sb("A2", [128, D], F32)
            B2 = K.sb("B2", [128, D], F32)
            Brow = Buf("rows1")
            K.q_sp.dma(L1G[:], ln1g_d.partition_broadcast(128), outs=[Brow])
            K.q_sp.dma(L1B[:], ln1b_d.partition_broadcast(128), outs=[Brow])
            K.push()
            bcast_rows(G2row, Brow2, adaT[:, 40:48])
            bcast_rows(G1row, Brow, adaT[:, 16:24])
            bcast_rows(A2, Brow, sc2p)
            bcast_rows(B2, Brow, adaT[:, 24:32])
            tmpr = K.sb("tmpr", [128, D], F32)
            Btr = Buf()
            op(DVE, lambda: ve.tensor_tensor(out=tmpr[:], in0=L1B[:], in1=A2[:], op=ALU.mult), outs=[Btr], ins=[Brow])
            op(DVE, lambda: ve.tensor_tensor(out=B2[:], in0=B2[:], in1=tmpr[:], op=ALU.add), outs=[Brow], ins=[Brow, Btr])
            op(DVE, lambda: ve.tensor_tensor(out=A2[:], in0=A2[:], in1=L1G[:], op=ALU.mult), outs=[Brow], ins=[Brow])
            K.barrier()
            K.pop()
            if stop == 5:
                raise _Stop()
            xin = [K.sb(f"xin2{i}", [128, D], F32) for i in range(3)]
            Bxin = [Buf(), Buf(), Buf()]
            for i in range(2):
                K.q_sp.dma(xin[i][:], x_d[i * 128:(i + 1) * 128, :], outs=[Bxin[i]])
            z = [K.sb(f"z{i}", [128, D], F32) for i in range(2)]
            Bz = [Buf(), Buf()]
            zn = [K.sb(f"zn{i}", [128, D], F32) for i in range(2)]
            Bzn = [Buf(), Buf()]
            x1t = [K.sb(f"x1t{i}", [128, D], F32) for i in range(2)]
            Bx1t = [Buf(), Buf()]
            h2f = [K.sb(f"h2f{i}", [128, D], F32) for i in range(2)]
            Bh2f = [Buf(), Buf()]
            st = K.sb("bnst", [128, 2, 6], F32)
            mvA = K.sb("bnmv", [128, NT, 2], F32)
            rsA = K.sb("lnrs", [128, NT, 2], F32)
            Bstt = Buf("bnst")
            Bst_t = [Buf(f"st{i}") for i in range(NT)]
            def dA(i):
                p = i % 2
                tsl = slice(i * 128, (i + 1) * 128)
                yb = [4 * p, 4 * p + 1]
                for half in range(2):
                    for k in range(8):
                        op(PEe, lambda: te.matmul(PB[yb[half]][:, :], lhsT=mergedT[:, k, tsl], rhs=wout[:, k, half * 512:(half + 1) * 512],
                                                  start=(k == 0), stop=(k == 7)), outs=[BPB[yb[half]]], ins=[BmT[i // 4], Bwo])

            def dB(i):
                p = i % 2
                px = i % 3
                if i + 2 < NT:
                    K.q_sp.dma(xin[(i + 2) % 3][:], x_d[(i + 2) * 128:(i + 3) * 128, :], outs=[Bxin[(i + 2) % 3]])
                yb = [4 * p, 4 * p + 1]
                for half in range(2):
                    hs = slice(half * 512, (half + 1) * 512)
                    op(DVE, lambda: ve.tensor_tensor(out=z[p][:, hs], in0=PB[yb[half]][:, :], in1=G1row[:, hs], op=ALU.mult),
                       outs=[Bz[p]], ins=[BPB[yb[half]], Brow])
                op(DVE, lambda: ve.scalar_tensor_tensor(out=z[p][:], in0=xin[px][:], scalar=ALPHA, in1=z[p][:], op0=ALU.mult, op1=ALU.add),
                   outs=[Bz[p]], ins=[Bxin[px], Bz[p]])
                mv = mvA[:, i, :]
                rs = rsA[:, i, 0:1]
                nmr = rsA[:, i, 1:2]
                for half in range(2):
                    op(DVE, lambda: ve.bn_stats(out=st[:, half, :], in_=z[p][:, half * 512:(half + 1) * 512]), outs=[Bstt], ins=[Bz[p]])
                op(DVE, lambda: ve.bn_aggr(out=mv, in_=st[:].rearrange("p a b -> p (a b)")), outs=[Bst_t[i]], ins=[Bstt])
                op(DVE, lambda: ve.tensor_scalar(out=rs, in0=mv[:, 1:2], scalar1=LN_EPS, scalar2=None, op0=ALU.add), outs=[Bst_t[i]], ins=[Bst_t[i]])
                op(ACT, lambda: se.activation(out=rs, in_=rs, func=AF.Sqrt), outs=[Bst_t[i]], ins=[Bst_t[i]])
                op(DVE, lambda: ve.reciprocal(out=rs, in_=rs), outs=[Bst_t[i]], ins=[Bst_t[i]])
                op(DVE, lambda: ve.tensor_scalar(out=nmr, in0=mv[:, 0:1], scalar1=rs, scalar2=-1.0, op0=ALU.mult, op1=ALU.mult),
                   outs=[Bst_t[i]], ins=[Bst_t[i]])
                op(ACT, lambda: se.activation(out=zn[p][:], in_=z[p][:], func=AF.Identity, scale=rs, bias=nmr), outs=[Bzn[p]], ins=[Bz[p], Bst_t[i]])

            def dC(i):
                p = i % 2
                tsl = slice(i * 128, (i + 1) * 128)
                op(DVE, lambda: ve.tensor_tensor(out=h2f[p][:], in0=zn[p][:], in1=A2[:], op=ALU.mult), outs=[Bh2f[p]], ins=[Bzn[p], Brow])
                op(POOL, lambda: ge.tensor_tensor(out=h2tok[:, i, :], in0=h2f[p][:], in1=B2[:], op=ALU.add), outs=[Bh2k[i]], ins=[Bh2f[p], Brow])
                op(POOL, lambda: ge.tensor_tensor(out=x1t[p][:], in0=zn[p][:], in1=L1G[:], op=ALU.mult), outs=[Bx1t[p]], ins=[Bzn[p], Brow])
                op(POOL, lambda: ge.tensor_tensor(out=x1t[p][:], in0=x1t[p][:], in1=L1B[:], op=ALU.add), outs=[Bx1t[p]], ins=[Bx1t[p], Brow])
                K.q_sp.dma(x1_d[tsl, :], x1t[p][:], outs=[Bx1d[i]], ins=[Bx1t[p]])

            def dD(i):
                p = i % 2
                tsl = slice(i * 128, (i + 1) * 128)
                tb_ = 2 + 4 * p
                pbf = PB[tb_][:, :].bitcast(BF16)
                for c in range(8):
                    op(PEe, lambda: te.transpose(pbf[:, c * 128:(c + 1) * 128], h2tok[:, i, c * 128:(c + 1) * 128], ident_b[:]),
                       outs=[BPB[tb_]], ins=[Bh2k[i], Bc])
                op(ACT, lambda: se.copy(out=h2T[:, :, tsl], in_=pbf[:, :].rearrange("p (c t) -> p c t", c=8)), outs=[Bh2T[i]], ins=[BPB[tb_]])

            dA(0)
            dA(1)
            dB(0)
            for i in range(NT):
                if i + 1 < NT:
                    dB(i + 1)
                dC(i)
                if i + 2 < NT:
                    dA(i + 2)
                dD(i)
            dump("h2T", h2T[:], [128, 8, S], Bh2T, BF16)
            K.barrier()
            K.pop()
            if stop == 6:
                raise _Stop()
            K.release("mergedT")

        NS = 96
        I32 = mybir.dt.int32
        hs_d = nc.dram_tensor("hs_scratch", [NS * 128, D], BF16, kind="Internal").ap()
        ys_d = nc.dram_tensor("ys_scratch", [NS * 128, D], F32, kind="Internal").ap()
        Bhs = Buf("hs_d")
        Bys = [Buf(f"ys{s_}") for s_ in range(NS)]

        _breg = {}

        def idma(out, out_off, in_, in_off, outs, ins, bound=None):
            bval = NS * 128 - 1 if bound is None else bound
            if bval not in _breg:
                _breg[bval] = ge.to_reg(bval)
            I = K.pool
            for b_ in ins:
                I.wait(b_.w)
                for t in b_.wl:
                    I.wait(t)
            for b_ in outs:
                I.wait(b_.w)
                for t in b_.wl:
                    I.wait(t)
                for t in b_.r.values():
                    I.wait(t)
                for t in b_.rd:
                    I.wait(t)
            q = K.q_pl
            j = q.rr
            q.rr = (q.rr + 1) % len(q.sems)
            key = f"{q.name}{j}"
            if q.vals[j] > 0:
                I.wait(DTok(q.sems[j], q.vals[j], key))
            ins_ = ge.indirect_dma_start(out=out, out_offset=out_off, in_=in_, in_offset=in_off,
                                             bounds_check=_breg[bval], oob_is_err=False)
            ins_.then_inc(q.sems[j], 16)
            q.vals[j] += 16
            tok = DTok(q.sems[j], q.vals[j], key)
            for b_ in ins:
                b_.rd.append(tok)
            for b_ in outs:
                if isinstance(b_.w, DTok):
                    b_.wl.append(b_.w)
                b_.w = tok
                b_.r = {}
                b_.rd = []
            return tok

        if True:
            K.push()
            wr = K.sb("wr", [128, 8, 72], BF16)
            brow = K.sb("brow", [128, 72], F32)
            Bwr = Buf("wr")
            K.q_pl.dma(wr[:], wr_d.rearrange("(k p) n -> p k n", p=128), outs=[Bwr])
            K.q_sp.dma(brow[:], br_d.partition_broadcast(128), outs=[Bwr])
            MK = K.sb("MK", [128, 2, NT, 64], F32)
            W12 = K.sb("W12", [128, NT, 2], F32)
            Lall = K.sb("Lall", [128, NT, 72], F32)
            lgs = K.sb("lgs", [128, NT, 8], F32)
            gmk = K.sb("gmk", [128, NT, 8], F32)
            lem = K.sb("lem", [128, NT, 64], F32)
            lem2 = K.sb("lem2", [128, NT, 64], F32)
            sm = K.sb("sm", [128, 8, NT], F32)
            Br = Buf("router")
            RB = [0, 1, 2]
            for i in range(NT):
                tsl = slice(i * 128, (i + 1) * 128)
                bk = RB[i // 7]
                cs_ = (i % 7) * 72
                for k in range(8):
                    op(PEe, lambda: te.matmul(PB[bk][:, cs_:cs_ + 72], lhsT=h2T[:, k, tsl], rhs=wr[:, k, :], start=(k == 0), stop=(k == 7)),
                       outs=[BPB[bk]], ins=[Bh2T[i], Bwr])
            R = lambda fn, eng=DVE, extra=(): op(eng, fn, outs=[Br], ins=[Br] + list(extra))
            for g in range(3):
                n_ = min(7, NT - 7 * g)
                R(lambda: ve.tensor_tensor(out=Lall[:, 7 * g:7 * g + n_, :], in0=PB[RB[g]][:, 0:n_ * 72].rearrange("p (i c) -> p i c", c=72),
                                           in1=brow[:].unsqueeze(1).to_broadcast([128, n_, 72]), op=ALU.add), extra=[BPB[RB[g]], Bwr])
            gmax, gsum, g_w, m1, m2, dd, w1 = [sm[:, j, :] for j in range(7)]
            bc8 = lambda a_: a_.unsqueeze(2).to_broadcast([128, NT, 8])
            bc64 = lambda a_: a_.unsqueeze(2).to_broadcast([128, NT, 64])
            R(lambda: ve.tensor_reduce(out=gmax, in_=Lall[:, :, 0:8], axis=AX.X, op=ALU.max))
            R(lambda: ve.tensor_tensor(out=lgs[:], in0=Lall[:, :, 0:8], in1=bc8(gmax), op=ALU.subtract))
            R(lambda: se.activation(out=gmk[:], in_=lgs[:], func=AF.Exp), eng=ACT)
            R(lambda: ve.tensor_reduce(out=gsum, in_=gmk[:], axis=AX.X, op=ALU.add))
            R(lambda: ve.reciprocal(out=g_w, in_=gsum))
            R(lambda: ve.tensor_scalar(out=gmk[:], in0=lgs[:], scalar1=0.0, scalar2=None, op0=ALU.is_ge))
            R(lambda: ve.tensor_scalar(out=gmk[:], in0=gmk[:], scalar1=1.0, scalar2=BIG, op0=ALU.subtract, op1=ALU.mult))
            R(lambda: ve.tensor_tensor(out=lem[:].rearrange("p i (g j) -> p i g j", j=8), in0=Lall[:, :, 8:72].rearrange("p i (g j) -> p i g j", j=8),
                                       in1=gmk[:].unsqueeze(3).to_broadcast([128, NT, 8, 8]), op=ALU.add))
            R(lambda: ve.tensor_reduce(out=m1, in_=lem[:], axis=AX.X, op=ALU.max))
            R(lambda: ve.tensor_tensor(out=MK[:, 0, :, :], in0=lem[:], in1=bc64(m1), op=ALU.is_ge))
            R(lambda: ve.scalar_tensor_tensor(out=lem2[:].rearrange("p i e -> p (i e)"), in0=MK[:, 0, :, :].rearrange("p i e -> p (i e)"), scalar=-BIG,
                                              in1=lem[:].rearrange("p i e -> p (i e)"), op0=ALU.mult, op1=ALU.add))
            R(lambda: ve.tensor_reduce(out=m2, in_=lem2[:], axis=AX.X, op=ALU.max))
            R(lambda: ve.tensor_tensor(out=MK[:, 1, :, :], in0=lem2[:], in1=bc64(m2), op=ALU.is_ge))
            R(lambda: ve.tensor_tensor(out=dd, in0=m1, in1=m2, op=ALU.subtract))
            R(lambda: se.activation(out=w1, in_=dd, func=AF.Sigmoid), eng=ACT)
            R(lambda: ve.tensor_tensor(out=W12[:, :, 0], in0=w1, in1=g_w, op=ALU.mult))
            R(lambda: ve.tensor_tensor(out=W12[:, :, 1], in0=g_w, in1=W12[:, :, 0], op=ALU.subtract))
            K.barrier()
            K.release("h2T")
            dump("MK", MK[:], [128, 2, NT, 64], [Br])
            dump("W12", W12[:], [128, NT, 2], [Br])

            Ab = K.sb("Ab", [128, NT * 64], BF16)
            stri_b = K.sb("stri_b", [128, 128], BF16)
            ones_b = K.sb("ones_b", [128, 128], BF16)
            POS = K.sb("POS", [128, NT, 64], F32)
            TOT = K.sb("TOT", [128, NT, 64], F32)
            CAR = K.sb("CAR", [128, NT, 64], F32)
            TM = K.sb("TMd", [128, NT, 64], F32)
            cnt = K.sb("cnt", [128, 64], F32)
            pad = K.sb("pad", [128, 64], F32)
            cend = K.sb("cend", [128, 64], F32)
            base = K.sb("base", [128, 64], F32)
            one64 = K.sb("one64", [128, 64], F32)
            Pf = K.sb("Pf", [128, 2, NT], F32)
            Pi = K.sb("Pi", [128, 2, NT], I32)
            Bd = Buf("dispatch")
            Dd_ = lambda fn, eng=DVE, extra=(): op(eng, fn, outs=[Bd], ins=[Bd] + list(extra))
            Dd_(lambda: ge.memset(stri_b[:], 1.0), eng=POOL)
            Dd_(lambda: ge.affine_select(out=stri_b[:], in_=stri_b[:], compare_op=ALU.is_gt, fill=0.0, base=0, pattern=[[1, 128]],
                                         channel_multiplier=-1), eng=POOL)
            Dd_(lambda: ge.memset(ones_b[:], 1.0), eng=POOL)
            Dd_(lambda: ge.memset(one64[:], 1.0), eng=POOL)
            Dd_(lambda: ve.tensor_tensor(out=Ab[:], in0=MK[:, 0, :, :].rearrange("p i e -> p (i e)"), in1=MK[:, 1, :, :].rearrange("p i e -> p (i e)"),
                                         op=ALU.add), extra=[Br])
            for half in range(2):
                b1, b2 = nbank(0, 8), nbank(0, 8)
                hsl_ = slice(half * 512, (half + 1) * 512)
                op(PEe, lambda: te.matmul(PB[b1][:, :], lhsT=stri_b[:], rhs=Ab[:, hsl_], start=True, stop=True), outs=[BPB[b1]], ins=[Bd])
                op(PEe, lambda: te.matmul(PB[b2][:, :], lhsT=ones_b[:], rhs=Ab[:, hsl_], start=True, stop=True), outs=[BPB[b2]], ins=[Bd])
                Dd_(lambda: ve.tensor_copy(out=POS[:].rearrange("p i e -> p (i e)")[:, hsl_], in_=PB[b1][:, :]), extra=[BPB[b1]])
                Dd_(lambda: ve.tensor_copy(out=TOT[:].rearrange("p i e -> p (i e)")[:, hsl_], in_=PB[b2][:, :]), extra=[BPB[b2]])
            Dd_(lambda: ve.memset(CAR[:, 0, :], 0.0))
            for i in range(1, NT):
                Dd_(lambda: ve.tensor_tensor(out=CAR[:, i, :], in0=CAR[:, i - 1, :], in1=TOT[:, i - 1, :], op=ALU.add))
            Dd_(lambda: ve.tensor_tensor(out=cnt[:], in0=CAR[:, NT - 1, :], in1=TOT[:, NT - 1, :], op=ALU.add))
            Dd_(lambda: ve.memset(pad[:], 0.0))
            for k in range(16):
                Dd_(lambda: ve.scalar_tensor_tensor(out=pad[:], in0=cnt[:], scalar=128.0 * k, in1=pad[:], op0=ALU.is_gt, op1=ALU.add))
            Dd_(lambda: ve.tensor_scalar(out=pad[:], in0=pad[:], scalar1=128.0, scalar2=None, op0=ALU.mult))
            Dd_(lambda: ve.tensor_tensor_scan(out=cend[:], data0=one64[:], data1=pad[:], initial=0.0, op0=ALU.mult, op1=ALU.add))
            Dd_(lambda: ve.tensor_tensor(out=base[:], in0=cend[:], in1=pad[:], op=ALU.subtract))
            Dd_(lambda: ve.tensor_tensor(out=CAR[:], in0=CAR[:], in1=base[:].unsqueeze(1).to_broadcast([128, NT, 64]), op=ALU.add))
            Dd_(lambda: ve.tensor_tensor(out=POS[:], in0=POS[:], in1=CAR[:], op=ALU.add))
            for j in range(2):
                Dd_(lambda: ve.tensor_tensor(out=TM[:], in0=MK[:, j, :, :], in1=POS[:], op=ALU.mult), extra=[Br])
                Dd_(lambda: ve.tensor_reduce(out=Pf[:, j, :], in_=TM[:], axis=AX.X, op=ALU.add))
            Dd_(lambda: ve.tensor_copy(out=Pi[:], in_=Pf[:]))
            dump("Pf", Pf[:], [128, 2, NT], [Bd])
            dump("cend", cend[:], [128, 64], [Bd])
            sl_i = K.sb("sl_i", [128, NS], I32)
            slf = K.sb("slf", [128, NS], F32)
            pcol_i = K.sb("pcol_i", [128, 1], I32)
            pcol = K.sb("pcol", [128, 1], F32)
            Erow = K.sb("Erow", [128, NS], F32)
            idxW = K.sb("idxW", [128, NS], I32)
            Dd_(lambda: ge.iota(out=sl_i[:], pattern=[[128, NS]], base=0, channel_multiplier=0), eng=POOL)
            Dd_(lambda: ge.iota(out=pcol_i[:], pattern=[[0, 1]], base=0, channel_multiplier=1), eng=POOL)
            Dd_(lambda: ve.tensor_copy(out=slf[:], in_=sl_i[:]))
            Dd_(lambda: ve.tensor_copy(out=pcol[:], in_=pcol_i[:]))
            K.push()
            cmp_ = K.sb("cmp_", [128, 32, 64], F32)
            for c3 in range(NS // 32):
                csl = slice(c3 * 32, (c3 + 1) * 32)
                Dd_(lambda: ve.tensor_tensor(out=cmp_[:], in0=cend[:].unsqueeze(1).to_broadcast([128, 32, 64]),
                                             in1=slf[:, csl].unsqueeze(2).to_broadcast([128, 32, 64]), op=ALU.is_le))
                Dd_(lambda: ve.tensor_reduce(out=Erow[:, csl], in_=cmp_[:], axis=AX.X, op=ALU.add))
            Dd_(lambda: ve.tensor_scalar(out=Erow[:], in0=Erow[:], scalar1=128.0, scalar2=pcol[:, 0:1], op0=ALU.mult, op1=ALU.add))
            Dd_(lambda: ve.tensor_copy(out=idxW[:], in_=Erow[:]))
            K.barrier()
            K.pop()
            dump("idxW", idxW[:], [128, NS], [Bd], I32)

            Bhs_l = [Buf(f"hs{i}") for i in range(2 * NT)]
            for i in range(NT):
                for j in range(2):
                    idma(hs_d[:, :], bass.IndirectOffsetOnAxis(ap=Pi[:, j, i:i + 1], axis=0), h2tok[:, i, :], None,
                         outs=[Bhs_l[2 * i + j]], ins=[Bh2k[i], Bd])
            if stop == 6.5:
                K.barrier()
                raise _Stop()

            NWB = 6
            K.push()
            wg = [K.sb(f"wg{i}", [128, 8, 256], BF16) for i in range(NWB)]
            wu = [K.sb(f"wu{i}", [128, 8, 256], BF16) for i in range(NWB)]
            wd = [K.sb(f"wd{i}", [128, 2, D], BF16) for i in range(NWB)]
            Bwg = [Buf() for _ in range(NWB)]
            Bwu = [Buf() for _ in range(NWB)]
            Bwd = [Buf() for _ in range(NWB)]
            NHB = 4
            hsl = [K.sb(f"hsl{i}", [128, D], BF16) for i in range(NHB)]
            Bhsl = [Buf() for _ in range(NHB)]
            hsT = [K.sb(f"hsT{i}", [128, 8, 128], BF16) for i in range(2)]
            BhsT = [Buf(), Buf()]
            sgs = [K.sb(f"sgs{i}", [128, 256], F32) for i in range(2)]
            Bsgs = [Buf(), Buf()]
            hid = [K.sb(f"hid{i}", [128, 2, 128], BF16) for i in range(2)]
            Bhid = [Buf(), Buf()]
            ysl = [K.sb(f"ysl{i}", [128, D], F32) for i in range(2)]
            Bysl = [Buf(), Buf()]
            def load_w(s_):
                w_ = s_ % NWB
                off = bass.IndirectOffsetOnAxis(ap=idxW[:, s_:s_ + 1], axis=0)
                idma(wg[w_][:].rearrange("p k f -> p (k f)"), None, weg_d[:, :], off, outs=[Bwg[w_]], ins=[Bd], bound=NEXP * 128 - 1)
                idma(wu[w_][:].rearrange("p k f -> p (k f)"), None, weu_d[:, :], off, outs=[Bwu[w_]], ins=[Bd], bound=NEXP * 128 - 1)
                idma(wd[w_][:].rearrange("p k f -> p (k f)"), None, wed_d[:, :], off, outs=[Bwd[w_]], ins=[Bd], bound=NEXP * 128 - 1)

            def stageL(s_):
                h_ = s_ % NHB
                K.q_sp.dma(hsl[h_][:], hs_d[s_ * 128:(s_ + 1) * 128, :], outs=[Bhsl[h_]], ins=Bhs_l)

            def stageA(s_):
                p = s_ % 2
                h_ = s_ % NHB
                tb_ = p
                pbf = PB[tb_][:, :].bitcast(BF16)
                for c in range(8):
                    op(PEe, lambda: te.transpose(pbf[:, c * 128:(c + 1) * 128], hsl[h_][:, c * 128:(c + 1) * 128], ident_b[:]),
                       outs=[BPB[tb_]], ins=[Bhsl[h_], Bc])
                op(ACT, lambda: se.copy(out=hsT[p][:], in_=pbf[:, :].rearrange("p (c t) -> p c t", c=8)), outs=[BhsT[p]], ins=[BPB[tb_]])

            def stageB(s_):
                p = s_ % 2
                w_ = s_ % NWB
                bg, bu = 2 + p, 4 + p
                for fc in range(2):
                    fsl = slice(fc * 128, (fc + 1) * 128)
                    for k in range(8):
                        op(PEe, lambda: te.matmul(PB[bg][:, fsl], lhsT=wg[w_][:, k, fsl], rhs=hsT[p][:, k, :], start=(k == 0), stop=(k == 7)),
                           outs=[BPB[bg]], ins=[Bwg[w_], BhsT[p]])
                for fc in range(2):
                    fsl = slice(fc * 128, (fc + 1) * 128)
                    for k in range(8):
                        op(PEe, lambda: te.matmul(PB[bu][:, fsl], lhsT=wu[w_][:, k, fsl], rhs=hsT[p][:, k, :], start=(k == 0), stop=(k == 7)),
                           outs=[BPB[bu]], ins=[Bwu[w_], BhsT[p]])
                op(ACT, lambda: se.activation(out=sgs[p][:], in_=PB[bg][:, 0:256], func=AF.Silu), outs=[Bsgs[p]], ins=[BPB[bg]])
                op(DVE, lambda: ve.tensor_tensor(out=hid[p][:].rearrange("p a t -> p (a t)"), in0=PB[bu][:, 0:256], in1=sgs[p][:], op=ALU.mult),
                   outs=[Bhid[p]], ins=[BPB[bu], Bsgs[p]])

            def stageC(s_):
                p = s_ % 2
                w_ = s_ % NWB
                for half in range(2):
                    yb = 6 + half
                    for fc in range(2):
                        op(PEe, lambda: te.matmul(PB[yb][:, :], lhsT=hid[p][:, fc, :], rhs=wd[w_][:, fc, half * 512:(half + 1) * 512],
                                                  start=(fc == 0), stop=(fc == 1)), outs=[BPB[yb]], ins=[Bhid[p], Bwd[w_]])
                op(ACT, lambda: se.copy(out=ysl[p][:, 0:512], in_=PB[6][:, :]), outs=[Bysl[p]], ins=[BPB[6]])
                op(DVE, lambda: ve.tensor_copy(out=ysl[p][:, 512:1024], in_=PB[7][:, :]), outs=[Bysl[p]], ins=[BPB[7]])
                K.q_sp.dma(ys_d[s_ * 128:(s_ + 1) * 128, :], ysl[p][:], outs=[Bys[s_]], ins=[Bysl[p]])

            for s_ in range(min(NWB - 1, NS)):
                load_w(s_)
            for s_ in range(min(NHB - 1, NS)):
                stageL(s_)
            stageA(0)
            for s_ in range(NS):
                if s_ + NHB - 1 < NS:
                    stageL(s_ + NHB - 1)
                if s_ + 1 < NS:
                    stageA(s_ + 1)
                stageB(s_)
                if s_ >= 1:
                    stageC(s_ - 1)
                if s_ + NWB - 1 < NS:
                    load_w(s_ + NWB - 1)
            stageC(NS - 1)
            K.barrier()
            K.pop()
            K.release("h2tok")
            if stop == 7:
                raise _Stop()

            NFB = 3
            xin = [K.sb(f"x1in{i}", [128, D], F32) for i in range(NFB)]
            Bxin = [Buf() for _ in range(NFB)]
            g1 = [K.sb(f"g1_{i}", [128, D], F32) for i in range(NFB)]
            g2 = [K.sb(f"g2_{i}", [128, D], F32) for i in range(NFB)]
            Bg1 = [Buf() for _ in range(NFB)]
            Bg2 = [Buf() for _ in range(NFB)]

            def fetch(i):
                q_ = i % NFB
                K.q_sp.dma(xin[q_][:], x1_d[i * 128:(i + 1) * 128, :], outs=[Bxin[q_]], ins=[Bx1d[i]])
                idma(g1[q_][:, :], None, ys_d[:, :], bass.IndirectOffsetOnAxis(ap=Pi[:, 0, i:i + 1], axis=0), outs=[Bg1[q_]], ins=Bys + [Bd])
                idma(g2[q_][:, :], None, ys_d[:, :], bass.IndirectOffsetOnAxis(ap=Pi[:, 1, i:i + 1], axis=0), outs=[Bg2[q_]], ins=Bys + [Bd])

            for i in range(NFB - 1):
                fetch(i)
            z = [K.sb(f"z2{i}", [128, D], F32) for i in range(3)]
            Bz = [Buf(), Buf(), Buf()]
            ot = [K.sb(f"ot{i}", [128, D], F32) for i in range(2)]
            Bot = [Buf(), Buf()]
            st = K.sb("bnst2", [128, 2, 6], F32)
            mvA = K.sb("bnmv2", [128, NT, 2], F32)
            rsA = K.sb("lnrs2", [128, NT, 2], F32)
            Bstt = Buf("bnst2")
            Bst_t = [Buf(f"st2_{i}") for i in range(NT)]
            outs_t = []
            def f1(i):
                p = i % 2
                q_ = i % NFB
                op(ACT, lambda: se.activation(out=g1[q_][:], in_=g1[q_][:], func=AF.Identity, scale=W12[:, i, 0:1]),
                   outs=[Bg1[q_]], ins=[Bg1[q_], Br])
                op(DVE, lambda: ve.scalar_tensor_tensor(out=g1[q_][:], in0=g2[q_][:], scalar=W12[:, i, 1:2], in1=g1[q_][:], op0=ALU.mult, op1=ALU.add),
                   outs=[Bg1[q_]], ins=[Bg1[q_], Bg2[q_], Br])
                if i == 0:
                    dump("y0", g1[q_][:], [128, D], [Bg1[q_]])
                op(DVE, lambda: ve.tensor_tensor(out=z[i % 3][:], in0=g1[q_][:], in1=G2row[:], op=ALU.mult), outs=[Bz[i % 3]], ins=[Bg1[q_], Brow2])

            def f2(i):
                p = i % 2
                q_ = i % NFB
                if i + NFB - 1 < NT:
                    fetch(i + NFB - 1)
                op(DVE, lambda: ve.scalar_tensor_tensor(out=z[i % 3][:], in0=xin[q_][:], scalar=ALPHA, in1=z[i % 3][:], op0=ALU.mult, op1=ALU.add),
                   outs=[Bz[i % 3]], ins=[Bxin[q_], Bz[i % 3]])
                mv = mvA[:, i, :]
                rs = rsA[:, i, 0:1]
                nmr = rsA[:, i, 1:2]
                for half in range(2):
                    op(DVE, lambda: ve.bn_stats(out=st[:, half, :], in_=z[i % 3][:, half * 512:(half + 1) * 512]), outs=[Bstt], ins=[Bz[i % 3]])
                op(DVE, lambda: ve.bn_aggr(out=mv, in_=st[:].rearrange("p a b -> p (a b)")), outs=[Bst_t[i]], ins=[Bstt])
                op(DVE, lambda: ve.tensor_scalar(out=rs, in0=mv[:, 1:2], scalar1=LN_EPS, scalar2=None, op0=ALU.add), outs=[Bst_t[i]], ins=[Bst_t[i]])
                op(ACT, lambda: se.activation(out=rs, in_=rs, func=AF.Sqrt), outs=[Bst_t[i]], ins=[Bst_t[i]])
                op(DVE, lambda: ve.reciprocal(out=rs, in_=rs), outs=[Bst_t[i]], ins=[Bst_t[i]])
                op(DVE, lambda: ve.tensor_scalar(out=nmr, in0=mv[:, 0:1], scalar1=rs, scalar2=-1.0, op0=ALU.mult, op1=ALU.mult),
                   outs=[Bst_t[i]], ins=[Bst_t[i]])
                op(ACT, lambda: se.activation(out=z[i % 3][:], in_=z[i % 3][:], func=AF.Identity, scale=rs, bias=nmr), outs=[Bz[i % 3]], ins=[Bz[i % 3], Bst_t[i]])

            def f3(i):
                p = i % 2
                tsl = slice(i * 128, (i + 1) * 128)
                op(POOL, lambda: ge.tensor_tensor(out=ot[p][:], in0=z[i % 3][:], in1=L2G[:], op=ALU.mult), outs=[Bot[p]], ins=[Bz[i % 3], Brow2])
                op(POOL, lambda: ge.tensor_tensor(out=ot[p][:], in0=ot[p][:], in1=L2B[:], op=ALU.add), outs=[Bot[p]], ins=[Bot[p], Brow2])
                outs_t.append(K.q_sp.dma(out_d[tsl, :], ot[p][:], ins=[Bot[p]]))

            f1(0)
            for i in range(NT):
                if i + 1 < NT:
                    f1(i + 1)
                f2(i)
                if i >= 1:
                    f3(i - 1)
            f3(NT - 1)
            for t in outs_t + list(dbg_out.values()):
                K.sp.wait(t)
            K.barrier()
            K.pop()
    return nc


_NC_CACHE = {}


def make_in_maps(inputs):
    f = lambda a: np.ascontiguousarray(np.asarray(a, dtype=np.float32))
    x = f(inputs["x"])
    c = f(inputs["c"])
    lbl = f(inputs["hgrn_lb_logits"])
    lbl_l = np.concatenate([lbl[0].reshape(4, 128).T, lbl[1].reshape(4, 128).T], axis=1)
    shared = {
        "w_ada": f(inputs["w_ada"][0]),
        "b_adaT": f(inputs["b_ada"][0].reshape(48, 128).T),
        "w_in": f(inputs["w_in"][0]),
        "bff": f(inputs["b_fox_forget"][0]),
        "lbl": f(lbl_l),
        "nw": f(inputs["hgrn_norm_w"][0]),
        "w_up_fox": f(inputs["w_up_fox"][0]),
        "w_up_hgrn": f(inputs["w_up_hgrn"][0]),
        "w_out": f(inputs["w_out"][0]),
        "ln1_g": f(inputs["ln1_g"][0]),
        "ln1_b": f(inputs["ln1_b"][0]),
        "ln2_g": f(inputs["ln2_g"][0]),
        "ln2_b": f(inputs["ln2_b"][0]),
        "w_r": f(np.concatenate([inputs["w_router_group"][0], inputs["w_router_expert"][0]], axis=1)),
        "b_r": f(np.concatenate([inputs["b_router_group"][0], inputs["b_router_expert"][0]], axis=0)),
        "w_eg": f(np.asarray(inputs["w_expert_gate"][0]).reshape(NEXP, 8, 128, 256).transpose(0, 2, 1, 3).reshape(NEXP * 128, 2048)),
        "w_eu": f(np.asarray(inputs["w_expert_up"][0]).reshape(NEXP, 8, 128, 256).transpose(0, 2, 1, 3).reshape(NEXP * 128, 2048)),
        "w_ed": f(np.asarray(inputs["w_expert_down"][0]).reshape(NEXP, 2, 128, D).transpose(0, 2, 1, 3).reshape(NEXP * 128, 2048)),
    }
    maps = []
    for b in range(8):
        m = dict(shared)
        m["x"] = f(x[b])
        m["cT"] = f(c[b].reshape(8, 128).T)
        maps.append(m)
    return maps


def kernel(**inputs):
    if "nc" not in _NC_CACHE:
        _NC_CACHE["nc"] = build()
    nc = _NC_CACHE["nc"]
    in_maps = make_in_maps(inputs)
    res = run_bass_kernel_spmd(nc, in_maps, core_ids=list(range(8)))
    out = np.stack([np.asarray(r["out"], dtype=np.float32) for r in res.results], axis=0)
    return out
```
